# Optimizing a Trainium2 kernel written in Bass

```python
import math
import jax, jax.numpy as jnp
from jax import lax
import numpy as np

D_MODEL = 1024
BATCH = 4
SEQ = 8192
DEPTH = 4

CHUNK = 64
Q_BLOCK = 128
N_HEADS = 4
DA_HEAD = 64
DA_VDIM = 2 * DA_HEAD
RET_DK = 64
RET_DV = 128
ROPE_BASE = 10000.0
GLA_DK = 64
GLA_DV = 128
GLA_RANK = 16
GLA_TAU = 16.0
D_FF = 2752
EPS = 1e-6
N_BRANCH = 3

A_Q = N_HEADS * 2 * DA_HEAD
A_K = N_HEADS * 2 * DA_HEAD
A_V = N_HEADS * DA_VDIM
B_Q = N_HEADS * RET_DK
B_K = N_HEADS * RET_DK
B_V = N_HEADS * RET_DV
B_G = N_HEADS * RET_DV
C_Q = N_HEADS * GLA_DK
C_K = N_HEADS * GLA_DK
C_V = N_HEADS * GLA_DV
C_G = N_HEADS * GLA_DV
C_A = GLA_RANK
GATE_COLS = N_BRANCH * D_MODEL
IN_COLS = A_Q + A_K + A_V + B_Q + B_K + B_V + B_G + C_Q + C_K + C_V + C_G + C_A + GATE_COLS

kernel_name = "hybrid_diffattn_retention_gla_macaron"


def _rmsnorm(t, g):
    tf = t.astype(jnp.float32)
    y = tf * lax.rsqrt(jnp.mean(tf * tf, axis=-1, keepdims=True) + EPS)
    return (y * g.astype(jnp.float32)).astype(t.dtype)


def _swiglu(t, w1, w3, w2):
    return (jax.nn.silu(t @ w1) * (t @ w3)) @ w2


def _heads(t, h):
    b, s, w = t.shape
    return t.reshape(b, s, h, w // h).transpose(0, 2, 1, 3)


def _merge_heads(t):
    b, h, s, d = t.shape
    return t.transpose(0, 2, 1, 3).reshape(b, s, h * d)


def _to_chunks(t):
    b, h, s, d = t.shape
    return t.reshape(b, h, s // CHUNK, CHUNK, d).transpose(2, 0, 1, 3, 4)


def _from_chunks(t):
    n, b, h, c, d = t.shape
    return t.transpose(1, 2, 0, 3, 4).reshape(b, h, n * c, d)


def _rotary(t):
    s, d = t.shape[-2], t.shape[-1]
    inv = ROPE_BASE ** (-jnp.arange(0, d, 2, dtype=jnp.float32) / d)
    ang = jnp.arange(s, dtype=jnp.float32)[:, None] * inv[None, :]
    cos, sin = jnp.cos(ang), jnp.sin(ang)
    t1, t2 = t[..., : d // 2], t[..., d // 2:]
    return jnp.concatenate([t1 * cos - t2 * sin, t1 * sin + t2 * cos], axis=-1)


def _diff_attention(q, k, v, lam):
    b, h, _, s, dh = q.shape
    nb = s // Q_BLOCK
    qb = q.reshape(b, h, 2, nb, Q_BLOCK, dh).transpose(3, 0, 1, 2, 4, 5)
    key_chunk = jnp.arange(s) // CHUNK
    scale = dh ** -0.5

    def block(args):
        qi, bi = args
        q_chunk = (bi * Q_BLOCK + jnp.arange(Q_BLOCK)) // CHUNK
        mask = key_chunk[None, :] <= q_chunk[:, None]
        sc = jnp.einsum('bhmqd,bhmkd->bhmqk', qi, k).astype(jnp.float32) * scale
        p = jax.nn.softmax(jnp.where(mask, sc, -jnp.inf), axis=-1)
        w = p[:, :, 0] - lam * p[:, :, 1]
        return jnp.einsum('bhqk,bhkd->bhqd', w.astype(v.dtype), v)

    out = lax.map(block, (qb, jnp.arange(nb)))
    return out.transpose(1, 2, 0, 3, 4).reshape(b, h, s, v.shape[-1])


def _retention(q, k, v):
    b, h, _, dk = q.shape
    log_g = jnp.log1p(-jnp.exp2(-5.0 - jnp.arange(h, dtype=jnp.float32)))
    pos = jnp.arange(CHUNK, dtype=jnp.float32)
    causal = pos[:, None] >= pos[None, :]
    d_in = jnp.exp(jnp.where(causal, log_g[:, None, None] * (pos[:, None] - pos[None, :]), -jnp.inf))
    q_dec = jnp.exp(log_g[:, None] * (pos + 1.0))[..., None]
    k_dec = jnp.exp(log_g[:, None] * (CHUNK - 1.0 - pos))[..., None]
    c_dec = jnp.exp(log_g * CHUNK)[:, None, None]

    def step(state, inp):
        qc, kc, vc = inp
        sc = jnp.einsum('bhid,bhjd->bhij', qc, kc) * d_in
        inner = jnp.einsum('bhij,bhje->bhie', sc, vc)
        cross = jnp.einsum('bhid,bhde->bhie', qc, state) * q_dec
        state = state * c_dec + jnp.einsum('bhjd,bhje->bhde', kc * k_dec, vc)
        return state, inner + cross

    init = jnp.zeros((b, h, dk, v.shape[-1]), jnp.float32)
    _, out = lax.scan(step, init, (_to_chunks(q), _to_chunks(k), _to_chunks(v)))
    return _from_chunks(out)


def _gla(q, k, v, log_a):
    b, h, _, dk = q.shape
    pos = jnp.arange(CHUNK)
    causal = (pos[:, None] >= pos[None, :])[:, :, None]

    def step(state, inp):
        qc, kc, vc, ac = inp
        cum = jnp.cumsum(ac, axis=2)
        rel = jnp.exp(jnp.where(causal, cum[:, :, :, None, :] - cum[:, :, None, :, :], -jnp.inf))
        sc = jnp.einsum('bhid,bhjd,bhijd->bhij', qc, kc, rel)
        inner = jnp.einsum('bhij,bhje->bhie', sc, vc)
        cross = jnp.einsum('bhid,bhde->bhie', qc * jnp.exp(cum), state)
        last = cum[:, :, -1:, :]
        state = state * jnp.exp(last[:, :, 0, :, None]) + jnp.einsum('bhjd,bhje->bhde', kc * jnp.exp(last - cum), vc)
        return state, inner + cross

    init = jnp.zeros((b, h, dk, v.shape[-1]), jnp.float32)
    _, out = lax.scan(step, init, (_to_chunks(q), _to_chunks(k), _to_chunks(v), _to_chunks(log_a)))
    return _from_chunks(out)


def _mixer(h, w_in, lam_qk, lam_init, da_g, ret_g, gla_a2, gla_ab, gla_g, w_ba, w_bb, w_bc, w_o):
    f32 = jnp.float32
    dt = h.dtype
    b, s, _ = h.shape
    proj = h @ w_in
    sizes = [A_Q, A_K, A_V, B_Q, B_K, B_V, B_G, C_Q, C_K, C_V, C_G, C_A, GATE_COLS]
    idx = np.cumsum(sizes)[:-1].tolist()
    aq, ak, av, rq, rk, rv, rg, cq, ck, cv, cg, ca, gates = jnp.split(proj, idx, axis=-1)

    def two_maps(t):
        return t.reshape(b, s, N_HEADS, 2, DA_HEAD).transpose(0, 2, 3, 1, 4)
    lq = lam_qk.astype(f32)
    lam = jnp.exp(jnp.sum(lq[0] * lq[1])) - jnp.exp(jnp.sum(lq[2] * lq[3])) + lam_init
    ya = _diff_attention(two_maps(aq), two_maps(ak), _heads(av, N_HEADS), lam)
    ya = _merge_heads(_rmsnorm(ya, da_g) * (1.0 - lam_init)).astype(dt)

    rq_h = _rotary(_heads(rq, N_HEADS).astype(f32))
    rk_h = _rotary(_heads(rk, N_HEADS).astype(f32)) * (RET_DK ** -0.5)
    yb = _retention(rq_h, rk_h, _heads(rv, N_HEADS).astype(f32))
    yb = jax.nn.silu(rg) * _merge_heads(_rmsnorm(yb, ret_g)).astype(dt)

    log_a = jax.nn.log_sigmoid((ca @ gla_a2 + gla_ab).astype(f32)) / GLA_TAU
    yc = _gla(_heads(cq, N_HEADS).astype(f32) * (GLA_DK ** -0.5), _heads(ck, N_HEADS).astype(f32),
              _heads(cv, N_HEADS).astype(f32), _heads(log_a, N_HEADS))
    yc = jax.nn.silu(cg) * _merge_heads(_rmsnorm(yc, gla_g)).astype(dt)

    g = jax.nn.sigmoid(gates).reshape(b, s, N_BRANCH, D_MODEL)
    merged = g[:, :, 0] * (ya @ w_ba) + g[:, :, 1] * (yb @ w_bb) + g[:, :, 2] * (yc @ w_bc)
    return merged @ w_o


def setup_inputs(seed: int = 0) -> dict:
    key = jax.random.key(seed)
    ks = jax.random.split(key, 24)
    f32 = jnp.float32
    L = DEPTH

    def nrm(k, shape, scale):
        return jax.random.normal(k, shape, f32) * scale

    def gain(k, shape):
        return 1.0 + 0.02 * jax.random.normal(k, shape, f32)

    return {
        'x': nrm(ks[0], (BATCH, SEQ, D_MODEL), 1.0),
        'ffn1_norm': gain(ks[1], (L, D_MODEL)),
        'ffn1_w1': nrm(ks[2], (L, D_MODEL, D_FF), D_MODEL ** -0.5),
        'ffn1_w3': nrm(ks[3], (L, D_MODEL, D_FF), D_MODEL ** -0.5),
        'ffn1_w2': nrm(ks[4], (L, D_FF, D_MODEL), D_FF ** -0.5),
        'mix_norm': gain(ks[5], (L, D_MODEL)),
        'w_in': nrm(ks[6], (L, D_MODEL, IN_COLS), D_MODEL ** -0.5),
        'lam_qk': nrm(ks[7], (L, 4, DA_HEAD), 0.1),
        'da_norm': gain(ks[8], (L, DA_VDIM)),
        'ret_norm': gain(ks[9], (L, RET_DV)),
        'gla_a2': nrm(ks[10], (L, GLA_RANK, C_K), GLA_RANK ** -0.5),
        'gla_a_bias': nrm(ks[11], (L, C_K), 0.1),
        'gla_norm': gain(ks[12], (L, GLA_DV)),
        'w_branch_a': nrm(ks[13], (L, A_V, D_MODEL), A_V ** -0.5),
        'w_branch_b': nrm(ks[14], (L, B_V, D_MODEL), B_V ** -0.5),
        'w_branch_c': nrm(ks[15], (L, C_V, D_MODEL), C_V ** -0.5),
        'w_out': nrm(ks[16], (L, D_MODEL, D_MODEL), D_MODEL ** -0.5),
        'ffn2_norm': gain(ks[17], (L, D_MODEL)),
        'ffn2_w1': nrm(ks[18], (L, D_MODEL, D_FF), D_MODEL ** -0.5),
        'ffn2_w3': nrm(ks[19], (L, D_MODEL, D_FF), D_MODEL ** -0.5),
        'ffn2_w2': nrm(ks[20], (L, D_FF, D_MODEL), D_FF ** -0.5),
        'final_norm': gain(ks[21], (D_MODEL,)),
    }


def reference(x, ffn1_norm, ffn1_w1, ffn1_w3, ffn1_w2, mix_norm, w_in, lam_qk, da_norm, ret_norm,
              gla_a2, gla_a_bias, gla_norm, w_branch_a, w_branch_b, w_branch_c, w_out,
              ffn2_norm, ffn2_w1, ffn2_w3, ffn2_w2, final_norm):
    for l in range(DEPTH):
        lam_init = 0.8 - 0.6 * math.exp(-0.3 * l)
        x = x + 0.5 * _swiglu(_rmsnorm(x, ffn1_norm[l]), ffn1_w1[l], ffn1_w3[l], ffn1_w2[l])
        h = _rmsnorm(x, mix_norm[l])
        x = x + _mixer(h, w_in[l], lam_qk[l], lam_init, da_norm[l], ret_norm[l], gla_a2[l],
                       gla_a_bias[l], gla_norm[l], w_branch_a[l], w_branch_b[l], w_branch_c[l], w_out[l])
        x = x + 0.5 * _swiglu(_rmsnorm(x, ffn2_norm[l]), ffn2_w1[l], ffn2_w3[l], ffn2_w2[l])
    return _rmsnorm(x, final_norm)
```

```python
import math, contextlib
import numpy as np
import concourse.bass as bass, concourse.mybir as mybir
from concourse.bass_utils import run_bass_kernel_spmd
from concourse.alu_op_type import AluOpType as ALU

F32, BF16 = mybir.dt.float32, mybir.dt.bfloat16
AF = mybir.ActivationFunctionType
D = 1024; DFF = 2752; NH = 4; IN_COLS = 7696; EPS = 1e-6
STRICT_SAME = True

WSPEC = [('ffn1_norm', (D,)), ('ffn1_w1', (D, DFF)), ('ffn1_w3', (D, DFF)), ('ffn1_w2', (DFF, D)),
         ('mix_norm', (D,)), ('w_in', (D, IN_COLS)), ('lam_qk', (4, 64)), ('da_norm', (128,)),
         ('ret_norm', (128,)), ('gla_a2', (16, 256)), ('gla_a_bias', (256,)), ('gla_norm', (128,)),
         ('w_branch_a', (512, D)), ('w_branch_b', (512, D)), ('w_branch_c', (512, D)), ('w_out', (D, D)),
         ('ffn2_norm', (D,)), ('ffn2_w1', (D, DFF)), ('ffn2_w3', (D, DFF)), ('ffn2_w2', (DFF, D))]


class Lane:
    def __init__(s, nc, es, name):
        s.sem = es.enter_context(nc.semaphore(name)); s.cnt = 0; s.name = name


class Eng(Lane):
    def __init__(s, nc, es, name, h, nlanes=0):
        super().__init__(nc, es, name)
        s.h = h; s.seen = {}
        s.lanes = [Lane(nc, es, f"{name}_l{i}") for i in range(nlanes)]; s.li = 0


class Buf:
    __slots__ = ('w', 'r')

    def __init__(s):
        s.w = None; s.r = {}


class K:
    def __init__(s, nc, es):
        s.nc = nc
        s.pe = Eng(nc, es, "pe", nc.tensor)
        s.act = Eng(nc, es, "act", nc.scalar)
        s.dve = Eng(nc, es, "dve", nc.vector)
        s.pool = Eng(nc, es, "pool", nc.gpsimd, nlanes=6)
        s.sp = Eng(nc, es, "sp", nc.sync, nlanes=10)
        s.engs = [s.pe, s.act, s.dve, s.pool, s.sp]
        s.dram = {}

    def dbuf(s, key):
        b = s.dram.get(key)
        if b is None:
            b = s.dram[key] = Buf()
        return b

    def _waits(s, eng, r, w, extra=()):
        need = {}

        def add(t):
            if t is None: return
            l, c = t
            if need.get(l, 0) < c: need[l] = c
        for b in r: add(b.w)
        for b in w:
            add(b.w)
            for l, c in b.r.items(): add((l, c))
        for t in extra: add(t)
        for l, c in need.items():
            if l is eng and (not STRICT_SAME or c > eng.cnt): continue
            if eng.seen.get(l, 0) >= c: continue
            eng.h.wait_ge(l.sem, c); eng.seen[l] = c

    def op(s, eng, fn, r=(), w=(), inc=True):
        s._waits(eng, r, w)
        inst = fn()
        tgt = eng.cnt + 1
        if inc:
            inst.then_inc(eng.sem, 1); eng.cnt = tgt
        for b in r: b.r[eng] = tgt
        for b in w:
            b.w = (eng, tgt); b.r = {}
        return inst

    def dma(s, eng, out, in_, r=(), w=(), **kw):
        lane = eng.lanes[eng.li]; eng.li = (eng.li + 1) % len(eng.lanes)
        s._waits(eng, r, w, extra=[(lane, lane.cnt)] if lane.cnt else ())
        eng.h.dma_start(out=out, in_=in_, **kw).then_inc(lane.sem, 16)
        lane.cnt += 16
        for b in r: b.r[lane] = lane.cnt
        for b in w:
            b.w = (lane, lane.cnt); b.r = {}

    def barrier(s):
        alll = []
        for e in s.engs:
            alll.append(e); alll.extend(e.lanes)
        for e in s.engs:
            for l in alll:
                if l is e or l.cnt == 0: continue
                if e.seen.get(l, 0) >= l.cnt: continue
                e.h.wait_ge(l.sem, l.cnt); e.seen[l] = l.cnt


def lam_init_of(l):
    return 0.8 - 0.6 * math.exp(-0.3 * l)


def build(S=8192, DEPTH=4, PH=None):
    NT = S // 128; NG = S // 512
    nc = bass.Bass("TRN2", target_bir_lowering=False)

    def dr(n, sh, dt=F32, kind="ExternalInput"):
        return nc.dram_tensor(n, list(sh), dt, kind=kind).ap()
    x_in = dr("x", [S, D])
    W = {n: dr(n, (DEPTH,) + sh) for n, sh in WSPEC}
    fin_g = dr("final_norm", [D])
    c_ident = dr("c_ident", [128, 128])
    c_rope = dr("c_rope", [128, NT, 64])
    c_rtab = dr("c_rtab", [128, 512])
    c_tri = dr("c_tri", [128, 128])
    c_mask = dr("c_mask", [128, 128])
    c_rdec = dr("c_rdec", [128, 2])
    y_out = dr("y", [S, D], kind="ExternalOutput")
    xres = dr("xres", [S, D], kind="Internal")
    qT_d = dr("qT_d", [4, 128, S], BF16, kind="Internal")
    kT_d = dr("kT_d", [4, 128, S], BF16, kind="Internal")
    v_d = dr("v_d", [128, NT, 4, 129], BF16, kind="Internal")
    ya_d = dr("ya_d", [128, NT, 512], BF16, kind="Internal")
    ybc_d = dr("ybc_d", [128, NT, 1024], BF16, kind="Internal")

    es = contextlib.ExitStack()
    with es:
        k = K(nc, es)
        pe, act, dve, pool, sp = k.pe, k.act, k.dve, k.pool, k.sp
        T, V, A, G = nc.tensor, nc.vector, nc.scalar, nc.gpsimd

        uid = [0]

        def sbt(stack, n, sh, dt):
            uid[0] += 1
            return stack.enter_context(nc.sbuf_tensor(f"{n}_{uid[0]}", list(sh), dt))
        ps = [es.enter_context(nc.psum_tensor(f"ps{i}", [128, 512], F32)) for i in range(8)]
        psb = [Buf() for _ in range(8)]
        identb = sbt(es, "identb", [128, 128], BF16); b_id = Buf()
        mh = sbt(es, "mh", [128, 1], F32); b_mh = Buf()
        k.dma(pool, identb[:], c_ident[:], w=[b_id])
        k.op(pool, lambda: G.memset(mh[:], -0.5), w=[b_mh])

        def mm(out, lhsT, rhs, r, w, start=True, stop=True, inc=True, **kw):
            return k.op(pe, lambda: T.matmul(out, lhsT, rhs, start=start, stop=stop, **kw), r=r, w=w, inc=inc)

        class NormPipe:
            def __init__(s, st, src, gain_ap, pst_banks):
                s.src = src
                s.xn = [sbt(st, f"xn{i}", [128, D], F32) for i in range(2)]; s.b_xn = [Buf(), Buf()]
                s.hb = [sbt(st, f"hb{i}", [128, D], BF16) for i in range(4)]; s.b_hb = [Buf() for _ in range(4)]
                s.hT = sbt(st, "hT", [128, 8, 512], BF16); s.b_hT = Buf()
                s.gB = sbt(st, "gB", [128, D], F32); s.b_gB = Buf()
                s.junk = sbt(st, "junk", [128, D], BF16); s.b_junk = Buf()
                s.ss = sbt(st, "nss", [128, 16], F32); s.b_ss = [Buf() for _ in range(4)]
                s.pst = pst_banks; s.n = 0
                k.dma(sp, s.gB[:], gain_ap.partition_broadcast(128), w=[s.b_gB])

            def part1(s, g):
                src_ap, src_key = s.src
                for t in range(4):
                    tt = g * 4 + t
                    xn = s.xn[t % 2]; bx = s.b_xn[t % 2]; ss = s.ss; bs = s.b_ss[t]
                    k.dma(sp, xn[:], src_ap[tt * 128:(tt + 1) * 128, :], r=[k.dbuf((src_key, tt))], w=[bx])
                    k.op(act, lambda: A.activation(out=s.junk[:], in_=xn[:], func=AF.Square, accum_out=ss[:, 4 * t:4 * t + 1]),
                         r=[bx], w=[s.b_junk, bs])
                    k.op(dve, lambda: V.tensor_scalar(out=ss[:, 4 * t + 1:4 * t + 2], in0=ss[:, 4 * t:4 * t + 1], scalar1=1.0 / D,
                                                      scalar2=EPS, op0=ALU.mult, op1=ALU.add), r=[bs], w=[bs])
                    k.op(pool, lambda: G.tensor_tensor(out=ss[:, 4 * t + 2:4 * t + 3], in0=ss[:, 4 * t + 1:4 * t + 2], in1=mh[:, 0:1],
                                                       op=ALU.pow), r=[bs, b_mh], w=[bs])
                    k.op(dve, lambda: V.scalar_tensor_tensor(out=s.hb[t][:], in0=xn[:], scalar=ss[:, 4 * t + 2:4 * t + 3], in1=s.gB[:],
                                                             op0=ALU.mult, op1=ALU.mult), r=[bx, bs, s.b_gB], w=[s.b_hb[t]])

            def part2(s, g):
                for t in range(4):
                    bank = s.pst[s.n % len(s.pst)]; s.n += 1
                    pst = ps[bank][:].bitcast(BF16)
                    for c in range(8):
                        k.op(pe, lambda: T.transpose(pst[:, c * 128:(c + 1) * 128], s.hb[t][:, c * 128:(c + 1) * 128], identb[:]),
                             r=[s.b_hb[t], b_id], w=[psb[bank]], inc=(c == 7))
                    src = pst.rearrange("p (c t) -> p c t", c=8)
                    if t % 2 == 0:
                        k.op(dve, lambda: V.tensor_copy(out=s.hT[:, :, t * 128:(t + 1) * 128], in_=src), r=[psb[bank]], w=[s.b_hT])
                    else:
                        k.op(act, lambda: A.copy(out=s.hT[:, :, t * 128:(t + 1) * 128], in_=src), r=[psb[bank]], w=[s.b_hT])

        def wload(dst, src_rows_fn, nk, ncols, piece, bufs):
            npieces = (ncols + piece - 1) // piece
            for pi in range(npieces):
                c0 = pi * piece; c1 = min(ncols, c0 + piece)
                for c in range(nk):
                    src = src_rows_fn(c)
                    rows = src.shape[0]
                    k.dma(pool, dst[0:rows, c, c0:c1], src[:, c0:c1], w=[bufs[pi]])

        def ffn_phase(l, which, src, dst, final):
            pre = 'ffn1' if which == 1 else 'ffn2'
            with contextlib.ExitStack() as st:
                WS = sbt(st, "WS", [128, 8 * DFF * 2 + 22 * D], BF16)
                W1 = WS[:, 0:8 * DFF].rearrange("p (c f) -> p c f", c=8)
                W3 = WS[:, 8 * DFF:16 * DFF].rearrange("p (c f) -> p c f", c=8)
                W2 = WS[:, 16 * DFF:16 * DFF + 22 * D].rearrange("p (c f) -> p c f", c=22)
                PIECE = 1408
                b_w1 = [Buf(), Buf()]; b_w3 = [Buf(), Buf()]; b_w2 = [Buf()]
                w1d, w3d, w2d = W[pre + '_w1'], W[pre + '_w3'], W[pre + '_w2']
                for pi in range(2):
                    c0 = pi * PIECE; c1 = min(DFF, c0 + PIECE)
                    for (wd, Wt, bb) in ((w1d, W1, b_w1), (w3d, W3, b_w3)):
                        for c in range(8):
                            k.dma(pool, Wt[:, c, c0:c1], wd[l, c * 128:(c + 1) * 128, c0:c1], w=[bb[pi]])
                for c in range(22):
                    rows = 128 if c < 21 else 64
                    k.dma(pool, W2[0:rows, c, :], w2d[l, c * 128:c * 128 + rows, :], w=[b_w2[0]])
                npipe = NormPipe(st, src, W[pre + '_norm'][l, :], [6, 7])
                u = sbt(st, "u", [128, 22, 512], BF16); b_u = [Buf() for _ in range(22)]
                sa = [sbt(st, f"sa{i}", [128, 512], F32) for i in range(2)]; b_sa = [Buf(), Buf()]
                xr = [sbt(st, f"xr{i}", [128, D], F32) for i in range(2)]; b_xr = [Buf(), Buf()]
                if final:
                    gF = sbt(st, "gF", [128, D], F32); b_gF = Buf()
                    k.dma(sp, gF[:], fin_g.partition_broadcast(128), w=[b_gF])
                    fss = sbt(st, "fss", [128, 8], F32); b_fss = [Buf(), Buf()]
                src_ap, src_key = src
                dst_ap, dst_key = dst
                npipe.part1(0); npipe.part2(0)
                nxr = 0
                for g in range(NG):
                    for f in range(22):
                        fs = 128 if f < 21 else 64
                        pa = f % 2; pb = 2 + f % 2
                        pi = 0 if f * 128 < PIECE else 1
                        for kk in range(8):
                            mm(ps[pa][0:fs, :], W1[:, kk, f * 128:f * 128 + fs], npipe.hT[:, kk, :], r=[npipe.b_hT, b_w1[pi]],
                               w=[psb[pa]], start=(kk == 0), stop=(kk == 7), inc=(kk == 7))
                        for kk in range(8):
                            mm(ps[pb][0:fs, :], W3[:, kk, f * 128:f * 128 + fs], npipe.hT[:, kk, :], r=[npipe.b_hT, b_w3[pi]],
                               w=[psb[pb]], start=(kk == 0), stop=(kk == 7), inc=(kk == 7))
                        k.op(act, lambda: A.activation(out=sa[f % 2][0:fs, :], in_=ps[pa][0:fs, :], func=AF.Silu),
                             r=[psb[pa]], w=[b_sa[f % 2]])
                        k.op(dve, lambda: V.tensor_tensor(out=u[0:fs, f, :], in0=sa[f % 2][0:fs, :], in1=ps[pb][0:fs, :], op=ALU.mult),
                             r=[b_sa[f % 2], psb[pb]], w=[b_u[f]])
                        if f == 8 and g + 1 < NG:
                            npipe.part1(g + 1)
                    if g + 1 < NG:
                        npipe.part2(g + 1)
                    for t in range(4):
                        tt = g * 4 + t
                        xs = nxr % 2; nxr += 1
                        k.dma(sp, xr[xs][:], src_ap[tt * 128:(tt + 1) * 128, :], r=[k.dbuf((src_key, tt))], w=[b_xr[xs]])
                        for half in range(2):
                            py = 4 + half
                            for f in range(22):
                                fs = 128 if f < 21 else 64
                                mm(ps[py][:, :], u[0:fs, f, t * 128:(t + 1) * 128], W2[0:fs, f, half * 512:(half + 1) * 512],
                                   r=[b_u[f], b_w2[0]], w=[psb[py]], start=(f == 0), stop=(f == 21), inc=(f == 21))
                            k.op(dve, lambda: V.scalar_tensor_tensor(out=xr[xs][:, half * 512:(half + 1) * 512], in0=ps[py][:, :], scalar=0.5,
                                                                     in1=xr[xs][:, half * 512:(half + 1) * 512], op0=ALU.mult, op1=ALU.add),
                                 r=[psb[py], b_xr[xs]], w=[b_xr[xs]])
                        if not final:
                            k.dma(sp, dst_ap[tt * 128:(tt + 1) * 128, :], xr[xs][:], r=[b_xr[xs]], w=[k.dbuf((dst_key, tt))])
                        else:
                            k.op(act, lambda: A.activation(out=npipe.junk[:], in_=xr[xs][:], func=AF.Square, accum_out=fss[:, 4 * xs:4 * xs + 1]),
                                 r=[b_xr[xs]], w=[npipe.b_junk, b_fss[xs]])
                            k.op(dve, lambda: V.tensor_scalar(out=fss[:, 4 * xs + 1:4 * xs + 2], in0=fss[:, 4 * xs:4 * xs + 1], scalar1=1.0 / D,
                                                              scalar2=EPS, op0=ALU.mult, op1=ALU.add), r=[b_fss[xs]], w=[b_fss[xs]])
                            k.op(pool, lambda: G.tensor_tensor(out=fss[:, 4 * xs + 2:4 * xs + 3], in0=fss[:, 4 * xs + 1:4 * xs + 2],
                                                               in1=mh[:, 0:1], op=ALU.pow), r=[b_fss[xs], b_mh], w=[b_fss[xs]])
                            k.op(dve, lambda: V.scalar_tensor_tensor(out=xr[xs][:], in0=xr[xs][:], scalar=fss[:, 4 * xs + 2:4 * xs + 3],
                                                                     in1=gF[:], op0=ALU.mult, op1=ALU.mult),
                                 r=[b_xr[xs], b_fss[xs], b_gF], w=[b_xr[xs]])
                            k.dma(sp, y_out[tt * 128:(tt + 1) * 128, :], xr[xs][:], r=[b_xr[xs]], w=[k.dbuf(('y', tt))])
                k.barrier()

        def m1a_phase(l, src):
            with contextlib.ExitStack() as st:
                WA = sbt(st, "WA", [128, 8, 1536], BF16); b_wa = [Buf(), Buf(), Buf()]
                wload(WA, lambda c: W['w_in'][l, c * 128:(c + 1) * 128, 0:1536], 8, 1536, 512, b_wa)
                npipe = NormPipe(st, src, W['mix_norm'][l, :], [6, 7])
                qk = [sbt(st, f"qk{i}", [128, 512], BF16) for i in range(2)]; b_qk = [Buf(), Buf()]
                vs = [sbt(st, f"vs{i}", [128, 4, 4, 129], BF16) for i in range(2)]; b_vs = [Buf(), Buf()]
                for i in range(2):
                    k.op(pool, lambda: G.memset(vs[i][:], 1.0), w=[b_vs[i]])
                npipe.part1(0); npipe.part2(0)
                for g in range(NG):
                    for c in range(8):
                        pb_ = c % 2
                        for kk in range(8):
                            mm(ps[pb_][:, :], WA[:, kk, c * 128:(c + 1) * 128], npipe.hT[:, kk, :], r=[npipe.b_hT, b_wa[c // 4]],
                               w=[psb[pb_]], start=(kk == 0), stop=(kk == 7), inc=(kk == 7))
                        if c % 2 == 0:
                            k.op(act, lambda: A.copy(out=qk[pb_][:], in_=ps[pb_][:, :]), r=[psb[pb_]], w=[b_qk[pb_]])
                        else:
                            k.op(dve, lambda: V.tensor_copy(out=qk[pb_][:], in_=ps[pb_][:, :]), r=[psb[pb_]], w=[b_qk[pb_]])
                        dd, key = (qT_d, 'qT') if c < 4 else (kT_d, 'kT')
                        k.dma(sp, dd[c % 4, :, g * 512:(g + 1) * 512], qk[pb_][:], r=[b_qk[pb_]], w=[k.dbuf((key, c % 4, g))])
                        if c == 3 and g + 1 < NG:
                            npipe.part1(g + 1)
                    vsl = g % 2
                    for t in range(4):
                        pv = 2 + t % 2
                        for kk in range(8):
                            mm(ps[pv][:, :], npipe.hT[:, kk, t * 128:(t + 1) * 128], WA[:, kk, 1024:1536], r=[npipe.b_hT, b_wa[2]],
                               w=[psb[pv]], start=(kk == 0), stop=(kk == 7), inc=(kk == 7))
                        src_v = ps[pv][:, :].rearrange("p (h e) -> p h e", h=4)
                        if t % 2 == 0:
                            k.op(act, lambda: A.copy(out=vs[vsl][:, t, :, 0:128], in_=src_v), r=[psb[pv]], w=[b_vs[vsl]])
                        else:
                            k.op(dve, lambda: V.tensor_copy(out=vs[vsl][:, t, :, 0:128], in_=src_v), r=[psb[pv]], w=[b_vs[vsl]])
                    k.dma(sp, v_d[:, g * 4:(g + 1) * 4, :, :], vs[vsl][:], r=[b_vs[vsl]], w=[k.dbuf(('v', g))])
                    if g + 1 < NG:
                        npipe.part2(g + 1)
                k.barrier()

        def attn_phase(l):
            li = lam_init_of(l)
            with contextlib.ExitStack() as st:
                kT = sbt(st, "kT", [128, 4, S], BF16); b_kT = [Buf() for _ in range(4)]
                va = sbt(st, "va", [128, NT, 4, 129], BF16); b_va = Buf()
                qg = [sbt(st, f"qg{i}", [128, 4, 512], BF16) for i in range(2)]; b_qg = [Buf(), Buf()]
                Pt = [sbt(st, f"Pt{i}", [128, 512], BF16) for i in range(4)]; b_P = [Buf() for _ in range(4)]
                yag = [sbt(st, f"yag{i}", [128, 4, 512], BF16) for i in range(2)]; b_yag = [Buf(), Buf()]
                lq = sbt(st, "lq", [128, 256], F32); b_lq = Buf()
                lt = sbt(st, "lt", [128, 8], F32); b_lt = Buf()
                ljunk = sbt(st, "ljunk", [128, 64], F32); b_lj = Buf()
                gda = sbt(st, "gda", [128, 128], F32); b_gda = Buf()
                sm = sbt(st, "sm", [128, 8], F32); b_sm = Buf()
                tmp = sbt(st, "atmp", [128, 128], F32); b_tmp = Buf()
                o = sbt(st, "ao", [128, 128], F32); b_o = Buf()
                oj = sbt(st, "aoj", [128, 128], BF16); b_oj = Buf()
                for hd in range(4):
                    k.dma(sp, kT[:, hd, :], kT_d[hd, :, :], r=[k.dbuf(('kT', hd, g)) for g in range(NG)], w=[b_kT[hd]])
                k.dma(sp, va[:], v_d[:], r=[k.dbuf(('v', g)) for g in range(NG)], w=[b_va])
                k.dma(sp, lq[:], W['lam_qk'][l].rearrange("a b -> (a b)").partition_broadcast(128), w=[b_lq])
                k.dma(sp, gda[:], W['da_norm'][l, :].partition_broadcast(128), w=[b_gda])
                for i_ in range(2):
                    k.op(dve, lambda: V.tensor_tensor(out=ljunk[:], in0=lq[:, i_ * 128:i_ * 128 + 64], in1=lq[:, i_ * 128 + 64:i_ * 128 + 128],
                                                      op=ALU.mult), r=[b_lq], w=[b_lj])
                    k.op(act, lambda: A.activation(out=ljunk[:], in_=ljunk[:], func=AF.Copy, accum_out=lt[:, i_:i_ + 1]), r=[b_lj], w=[b_lj, b_lt])
                k.op(act, lambda: A.activation(out=lt[:, 2:4], in_=lt[:, 0:2], func=AF.Exp), r=[b_lt], w=[b_lt])
                k.op(dve, lambda: V.tensor_tensor(out=lt[:, 4:5], in0=lt[:, 3:4], in1=lt[:, 2:3], op=ALU.subtract), r=[b_lt], w=[b_lt])
                k.op(dve, lambda: V.tensor_scalar(out=lt[:, 5:6], in0=lt[:, 4:5], scalar1=-li, scalar2=None, op0=ALU.add), r=[b_lt], w=[b_lt])
                k.op(dve, lambda: V.tensor_scalar(out=gda[:], in0=gda[:], scalar1=(1.0 - li), scalar2=None, op0=ALU.mult), r=[b_gda], w=[b_gda])
                nP = 0
                for Gq in range(NG):
                    qs = Gq % 2
                    k.dma(sp, qg[qs][:], qT_d[:, :, Gq * 512:(Gq + 1) * 512].rearrange("h p s -> p h s"),
                          r=[k.dbuf(('qT', hd, Gq)) for hd in range(4)], w=[b_qg[qs]])
                    ys = Gq % 2
                    for hd in range(4):
                        nkb = 4 * (Gq + 1)
                        for kb in range(nkb):
                            dg = kb - 4 * Gq
                            t0 = max(dg, 0); q0 = t0 * 128
                            for m in range(2):
                                scb = 4 + (nP % 4); pslot = nP % 4; nP += 1
                                mm(ps[scb][:, q0:512], kT[m * 64:(m + 1) * 64, hd, kb * 128:(kb + 1) * 128],
                                   qg[qs][m * 64:(m + 1) * 64, hd, q0:512], r=[b_kT[hd], b_qg[qs]], w=[psb[scb]])
                                k.op(act, lambda: A.activation(out=Pt[pslot][:, q0:512], in_=ps[scb][:, q0:512], func=AF.Exp, scale=0.125),
                                     r=[psb[scb]], w=[b_P[pslot]])
                                if dg >= 0:
                                    k.op(pool, lambda: G.memset(Pt[pslot][64:128, q0:q0 + 64], 0.0), w=[b_P[pslot]])
                                for t in range(t0, 4):
                                    ab = m * 2 + t // 2
                                    acc = ps[ab][:, (t % 2) * 129:(t % 2) * 129 + 129]
                                    last = (kb == 4 * Gq + t)
                                    mm(acc, Pt[pslot][:, t * 128:(t + 1) * 128], va[:, kb, hd, :], r=[b_P[pslot], b_va], w=[psb[ab]],
                                       start=(kb == 0 and t % 2 == 0), stop=last, inc=(t == 3), skip_group_check=True)
                        for t in range(4):
                            a0 = ps[t // 2][:, (t % 2) * 129:(t % 2) * 129 + 129]
                            a1 = ps[2 + t // 2][:, (t % 2) * 129:(t % 2) * 129 + 129]
                            r0 = [psb[t // 2], psb[2 + t // 2]]
                            k.op(dve, lambda: V.reciprocal(out=sm[:, 0:1], in_=a0[:, 128:129]), r=r0, w=[b_sm])
                            k.op(dve, lambda: V.reciprocal(out=sm[:, 1:2], in_=a1[:, 128:129]), r=r0, w=[b_sm])
                            k.op(dve, lambda: V.tensor_tensor(out=sm[:, 2:3], in0=sm[:, 1:2], in1=lt[:, 5:6], op=ALU.mult), r=[b_sm, b_lt], w=[b_sm])
                            k.op(dve, lambda: V.tensor_scalar(out=tmp[:], in0=a1[:, 0:128], scalar1=sm[:, 2:3], scalar2=None, op0=ALU.mult),
                                 r=r0 + [b_sm], w=[b_tmp])
                            k.op(dve, lambda: V.scalar_tensor_tensor(out=o[:], in0=a0[:, 0:128], scalar=sm[:, 0:1], in1=tmp[:], op0=ALU.mult,
                                                                     op1=ALU.add), r=r0 + [b_sm, b_tmp], w=[b_o])
                            k.op(act, lambda: A.activation(out=oj[:], in_=o[:], func=AF.Square, accum_out=sm[:, 3:4]), r=[b_o], w=[b_oj, b_sm])
                            k.op(dve, lambda: V.tensor_scalar(out=sm[:, 4:5], in0=sm[:, 3:4], scalar1=1.0 / 128, scalar2=EPS, op0=ALU.mult,
                                                              op1=ALU.add), r=[b_sm], w=[b_sm])
                            k.op(pool, lambda: G.tensor_tensor(out=sm[:, 5:6], in0=sm[:, 4:5], in1=mh[:, 0:1], op=ALU.pow), r=[b_sm, b_mh], w=[b_sm])
                            k.op(dve, lambda: V.scalar_tensor_tensor(out=yag[ys][:, t, hd * 128:(hd + 1) * 128], in0=o[:], scalar=sm[:, 5:6],
                                                                     in1=gda[:], op0=ALU.mult, op1=ALU.mult),
                                 r=[b_o, b_sm, b_gda], w=[b_yag[ys]])
                    k.dma(sp, ya_d[:, Gq * 4:(Gq + 1) * 4, :], yag[ys][:], r=[b_yag[ys]], w=[k.dbuf(('ya', Gq))])
                k.barrier()

        def m1b_phase(l, src):
            NC_B = 3088
            with contextlib.ExitStack() as st:
                WB = sbt(st, "WB", [128, 8, NC_B], BF16); b_wb = [Buf() for _ in range(7)]
                offs = [0, 512, 1024, 1536, 2048, 2560, 3072, 3088]
                for pi in range(7):
                    for c in range(8):
                        k.dma(pool, WB[:, c, offs[pi]:offs[pi + 1]], W['w_in'][l, c * 128:(c + 1) * 128, 1536 + offs[pi]:1536 + offs[pi + 1]],
                              w=[b_wb[pi]])
                npipe = NormPipe(st, src, W['mix_norm'][l, :], [3])
                rope = sbt(st, "rope", [128, NT, 64], F32); b_rope = Buf()
                rtab = sbt(st, "rtab", [128, 512], F32); b_rtab = Buf()
                tri = sbt(st, "tri", [128, 128], F32); maskb = sbt(st, "maskb", [128, 128], F32); b_cst = Buf()
                rdec = sbt(st, "rdec", [128, 2], F32)
                a2a = sbt(st, "a2a", [32, 256], F32); b_a2 = Buf()
                gn = sbt(st, "gn", [128, 2, 128], F32); b_gn = Buf()
                k.dma(sp, rope[:], c_rope[:], w=[b_rope])
                k.dma(sp, rtab[:], c_rtab[:], w=[b_rtab])
                k.dma(sp, tri[:], c_tri[:], w=[b_cst]); k.dma(sp, maskb[:], c_mask[:], w=[b_cst]); k.dma(sp, rdec[:], c_rdec[:], w=[b_cst])
                k.op(pool, lambda: G.memset(a2a[:], 0.0), w=[b_a2])
                k.dma(sp, a2a[0:16, :], W['gla_a2'][l], w=[b_a2])
                k.dma(sp, a2a[16:17, :], W['gla_a_bias'][l:l + 1, :], w=[b_a2])
                k.dma(sp, gn[:, 0, :], W['ret_norm'][l, :].partition_broadcast(128), w=[b_gn])
                k.dma(sp, gn[:, 1, :], W['gla_norm'][l, :].partition_broadcast(128), w=[b_gn])
                caT = sbt(st, "caT", [32, 512], F32); b_caT = Buf()
                vb = sbt(st, "vb", [128, 2, 512], BF16); b_vb = [Buf(), Buf()]
                GG = sbt(st, "GG", [128, 2, 512], F32); b_GG = [Buf(), Buf()]
                rt = sbt(st, "rt", [128, 6, 256], F32); b_rt = Buf()
                qkr = sbt(st, "qkr", [128, 512], F32); b_qkr = Buf()
                qkg = sbt(st, "qkg", [128, 2, 512], BF16); b_qkg = [Buf(), Buf()]
                spz = sbt(st, "spz", [128, 256], F32); b_spz = Buf()
                E12 = sbt(st, "E12", [128, 512], F32); b_E = Buf()
                dec = sbt(st, "dec", [128, 2], F32); b_dec = Buf()
                qkT = sbt(st, "qkT", [128, 2, 4, 128], BF16); b_qkT = [Buf(), Buf()]
                scm = [sbt(st, f"scm{i}", [128, 128], BF16) for i in range(2)]; b_scm = [Buf(), Buf()]
                stf = sbt(st, "stf", [128, 4, 128], F32); b_stf = [Buf() for _ in range(4)]
                stb = sbt(st, "stb", [128, 4, 128], BF16); b_stb = [Buf() for _ in range(4)]
                oss = sbt(st, "oss", [128, 24], F32); b_oss = Buf()
                ojk = sbt(st, "ojk", [128, 128], BF16); b_ojk = Buf()
                ybc = [sbt(st, f"ybc{i}", [128, 1024], BF16) for i in range(2)]; b_ybc = [Buf(), Buf()]
                for i in range(4):
                    k.op(pool, lambda: G.memset(stf[:, i, :], 0.0), w=[b_stf[i]])
                    k.op(pool, lambda: G.memset(stb[:, i, :], 0.0), w=[b_stb[i]])
                k.op(pool, lambda: G.memset(caT[:], 1.0), w=[b_caT])
                npipe.part1(0); npipe.part2(0)
                npj = 0
                for g in range(NG):
                    hT = npipe.hT
                    for kk in range(8):
                        mm(ps[2][0:16, :], WB[:, kk, 3072:3088], hT[:, kk, :], r=[npipe.b_hT, b_wb[6]], w=[psb[2]], start=(kk == 0), stop=(kk == 7),
                           inc=(kk == 7))
                    k.op(dve, lambda: V.tensor_copy(out=caT[0:16, :], in_=ps[2][0:16, :]), r=[psb[2]], w=[b_caT])
                    for t in range(4):
                        tt = g * 4 + t
                        lhs = lambda kk: hT[:, kk, t * 128:(t + 1) * 128]

                        def proj(pi):
                            nonlocal npj
                            bank = npj % 2; npj += 1
                            for kk in range(8):
                                mm(ps[bank][:, :], lhs(kk), WB[:, kk, offs[pi]:offs[pi + 1]], r=[npipe.b_hT, b_wb[pi]], w=[psb[bank]],
                                   start=(kk == 0), stop=(kk == 7), inc=(kk == 7))
                            return bank
                        for mx, pi in ((0, 1), (1, 4)):
                            bk = proj(pi)
                            k.op(act, lambda: A.copy(out=vb[:, mx, :], in_=ps[bk][:, :]), r=[psb[bk]], w=[b_vb[mx]])
                        for mx, pi in ((0, 2), (1, 5)):
                            bk = proj(pi)
                            k.op(act, lambda: A.activation(out=GG[:, mx, :], in_=ps[bk][:, :], func=AF.Silu), r=[psb[bk]], w=[b_GG[mx]])
                            k.op(pool, lambda: G.tensor_tensor(out=GG[:, mx, :].rearrange("p (h e) -> p h e", h=4),
                                                               in0=GG[:, mx, :].rearrange("p (h e) -> p h e", h=4),
                                                               in1=gn[:, mx, :].unsqueeze(1).broadcast_to([128, 4, 128]), op=ALU.mult),
                                 r=[b_GG[mx], b_gn], w=[b_GG[mx]])
                        bk = proj(0)
                        p3 = ps[bk][:, :].rearrange("p (a two d) -> p a two d", a=8, two=2)
                        cosb = rope[:, tt, 0:32].unsqueeze(1).broadcast_to([128, 8, 32])
                        sinb = rope[:, tt, 32:64].unsqueeze(1).broadcast_to([128, 8, 32])
                        r3 = lambda i: rt[:, i, :].rearrange("p (a d) -> p a d", a=8)
                        q3 = qkr[:].rearrange("p (a two d) -> p a two d", a=8, two=2)
                        rr_ = [psb[bk], b_rope]
                        k.op(dve, lambda: V.tensor_tensor(out=r3(0), in0=p3[:, :, 0, :], in1=cosb, op=ALU.mult), r=rr_, w=[b_rt])
                        k.op(dve, lambda: V.tensor_tensor(out=r3(1), in0=p3[:, :, 1, :], in1=sinb, op=ALU.mult), r=rr_, w=[b_rt])
                        k.op(dve, lambda: V.tensor_tensor(out=r3(2), in0=p3[:, :, 0, :], in1=sinb, op=ALU.mult), r=rr_, w=[b_rt])
                        k.op(dve, lambda: V.tensor_tensor(out=r3(3), in0=p3[:, :, 1, :], in1=cosb, op=ALU.mult), r=rr_, w=[b_rt])
                        k.op(dve, lambda: V.tensor_tensor(out=q3[:, :, 0, :], in0=r3(0), in1=r3(1), op=ALU.subtract), r=[b_rt], w=[b_qkr])
                        k.op(dve, lambda: V.tensor_tensor(out=q3[:, :, 1, :], in0=r3(2), in1=r3(3), op=ALU.add), r=[b_rt], w=[b_qkr])
                        k.op(dve, lambda: V.tensor_tensor(out=qkg[:, 0, :], in0=qkr[:], in1=rtab[:], op=ALU.mult), r=[b_qkr, b_rtab], w=[b_qkg[0]])
                        mm(ps[2][:, 0:256], caT[0:32, t * 128:(t + 1) * 128], a2a[0:32, :], r=[b_caT, b_a2], w=[psb[2]])
                        k.op(act, lambda: A.activation(out=spz[:], in_=ps[2][:, 0:256], func=AF.Exp, scale=-1.0), r=[psb[2]], w=[b_spz])
                        k.op(act, lambda: A.activation(out=spz[:], in_=spz[:], func=AF.Ln, bias=1.0), r=[b_spz], w=[b_spz])
                        mm(ps[2][:, 256:512], tri[:], spz[:], r=[b_cst, b_spz], w=[psb[2]])
                        for pr in range(2):
                            mm(ps[2][:, 2 * pr:2 * pr + 2], spz[:, pr * 128:(pr + 1) * 128], tri[:, 126:128], r=[b_cst, b_spz], w=[psb[2]],
                               inc=(pr == 1))
                        k.op(act, lambda: A.activation(out=E12[:, 0:256], in_=ps[2][:, 256:512], func=AF.Exp, bias=math.log(0.125)),
                             r=[psb[2]], w=[b_E])
                        k.op(act, lambda: A.activation(out=E12[:, 256:512], in_=ps[2][:, 256:512], func=AF.Exp, scale=-1.0), r=[psb[2]], w=[b_E])
                        k.op(act, lambda: A.activation(out=dec[:, 0:1], in_=ps[2][:, 1:2], func=AF.Exp), r=[psb[2]], w=[b_dec])
                        k.op(act, lambda: A.activation(out=dec[:, 1:2], in_=ps[2][:, 3:4], func=AF.Exp), r=[psb[2]], w=[b_dec])
                        bk = proj(3)
                        k.op(dve, lambda: V.tensor_tensor(out=qkg[:, 1, :], in0=ps[bk][:, :], in1=E12[:], op=ALU.mult), r=[psb[bk], b_E], w=[b_qkg[1]])
                        ysl = tt % 2
                        for mx in range(2):
                            tb = ps[3][:, mx * 256:(mx + 1) * 256].bitcast(BF16)
                            for j in range(4):
                                k.op(pe, lambda: T.transpose(tb[:, j * 128:(j + 1) * 128], qkg[:, mx, j * 128:(j + 1) * 128], identb[:]),
                                     r=[b_qkg[mx], b_id], w=[psb[3]], inc=(j == 3))
                            k.op(act, lambda: A.copy(out=qkT[:, mx, :, :].rearrange("p a b -> p (a b)"), in_=tb), r=[psb[3]], w=[b_qkT[mx]])
                            scb = 4 + mx; outb = 6; dsb = 7
                            for hd in range(4):
                                pr, hp = hd // 2, hd % 2
                                R = slice(hp * 64, hp * 64 + 64)
                                si = mx * 2 + pr
                                mm(ps[scb][:, hd * 128:(hd + 1) * 128], qkT[R, mx, 2 + pr, :], qkT[R, mx, pr, :], r=[b_qkT[mx]], w=[psb[scb]])
                                sl = hd % 2
                                k.op(dve, lambda: V.tensor_tensor(out=scm[sl][:], in0=ps[scb][:, hd * 128:(hd + 1) * 128], in1=maskb[:], op=ALU.mult),
                                     r=[psb[scb], b_cst], w=[b_scm[sl]])
                                vv = vb[:, mx, hd * 128:(hd + 1) * 128]
                                mm(ps[outb][:, hd * 128:(hd + 1) * 128], scm[sl][:], vv, r=[b_scm[sl], b_vb[mx]], w=[psb[outb]], start=True, stop=False,
                                   inc=False)
                                mm(ps[outb][:, hd * 128:(hd + 1) * 128], qkT[R, mx, pr, :], stb[R, si, :], r=[b_qkT[mx], b_stb[si]], w=[psb[outb]],
                                   start=False, stop=True)
                                mm(ps[dsb][R, si * 128:(si + 1) * 128], qkg[:, mx, 256 + hd * 64:256 + hd * 64 + 64], vv, r=[b_qkg[mx], b_vb[mx]],
                                   w=[psb[dsb]])
                                if hp == 1:
                                    dcol = rdec[:, pr:pr + 1] if mx == 0 else dec[:, pr:pr + 1]
                                    rd = [b_cst] if mx == 0 else [b_dec]
                                    k.op(dve, lambda: V.tensor_tensor(out=stf[:, si, :], in0=ps[dsb][:, si * 128:(si + 1) * 128], in1=stf[:, si, :],
                                                                      op=ALU.add), r=[psb[dsb], b_stf[si]], w=[b_stf[si]])
                                    k.op(dve, lambda: V.tensor_scalar(out=stf[:, si, :], in0=stf[:, si, :], scalar1=dcol, scalar2=None, op0=ALU.mult),
                                         r=[b_stf[si]] + rd, w=[b_stf[si]])
                                    k.op(pool, lambda: G.tensor_copy(out=stb[:, si, :], in_=stf[:, si, :]), r=[b_stf[si]], w=[b_stb[si]])
                            for hd in range(4):
                                col = (mx * 4 + hd) * 3
                                osl = ps[outb][:, hd * 128:(hd + 1) * 128]
                                k.op(act, lambda: A.activation(out=ojk[:], in_=osl, func=AF.Square, accum_out=oss[:, col:col + 1]), r=[psb[outb]],
                                     w=[b_ojk, b_oss])
                                k.op(dve, lambda: V.tensor_scalar(out=oss[:, col + 1:col + 2], in0=oss[:, col:col + 1], scalar1=1.0 / 128, scalar2=EPS,
                                                                  op0=ALU.mult, op1=ALU.add), r=[b_oss], w=[b_oss])
                                k.op(pool, lambda: G.tensor_tensor(out=oss[:, col + 2:col + 3], in0=oss[:, col + 1:col + 2], in1=mh[:, 0:1], op=ALU.pow),
                                     r=[b_oss, b_mh], w=[b_oss])
                                k.op(dve, lambda: V.scalar_tensor_tensor(out=ybc[ysl][:, mx * 512 + hd * 128:mx * 512 + (hd + 1) * 128], in0=osl,
                                                                         scalar=oss[:, col + 2:col + 3], in1=GG[:, mx, hd * 128:(hd + 1) * 128],
                                                                         op0=ALU.mult, op1=ALU.mult), r=[psb[outb], b_oss, b_GG[mx]], w=[b_ybc[ysl]])
                        k.dma(sp, ybc_d[:, tt, :], ybc[ysl][:], r=[b_ybc[ysl]], w=[k.dbuf(('ybc', tt))])
                        if t == 1 and g + 1 < NG:
                            npipe.part1(g + 1)
                    if g + 1 < NG:
                        npipe.part2(g + 1)
                k.barrier()

        def m1c_phase(l, src, dst):
            with contextlib.ExitStack() as st:
                WG = sbt(st, "WG", [128, 8, 3072], BF16); b_wg = [Buf() for _ in range(6)]
                WBR = sbt(st, "WBR", [128, 3, 4, D], BF16); b_wbr = [Buf() for _ in range(3)]
                WO = sbt(st, "WO", [128, 8, D], BF16); b_wo = Buf()
                for pi in range(6):
                    for c in range(8):
                        k.dma(pool, WG[:, c, pi * 512:(pi + 1) * 512], W['w_in'][l, c * 128:(c + 1) * 128, 4624 + pi * 512:4624 + (pi + 1) * 512],
                              w=[b_wg[pi]])
                for b, nm in enumerate(('w_branch_a', 'w_branch_b', 'w_branch_c')):
                    for c in range(4):
                        k.dma(pool, WBR[:, b, c, :], W[nm][l, c * 128:(c + 1) * 128, :], w=[b_wbr[b]])
                for c in range(8):
                    k.dma(pool, WO[:, c, :], W['w_out'][l, c * 128:(c + 1) * 128, :], w=[b_wo])
                npipe = NormPipe(st, src, W['mix_norm'][l, :], [5])
                yin = [sbt(st, f"yin{i}", [128, 1536], BF16) for i in range(2)]; b_yin = [Buf(), Buf()]
                yT = sbt(st, "yT", [128, 12, 128], BF16); b_yT = Buf()
                sg = [sbt(st, f"sg{i}", [128, 512], F32) for i in range(2)]; b_sg = [Buf(), Buf()]
                mg = sbt(st, "mg", [128, 512], F32); b_mg = Buf()
                mt = sbt(st, "mt", [128, 512], F32); b_mt = Buf()
                mb = sbt(st, "mb", [128, D], BF16); b_mb = Buf()
                mT = sbt(st, "mT", [128, 8, 128], BF16); b_mT = Buf()
                xr = [sbt(st, f"xr{i}", [128, D], F32) for i in range(2)]; b_xr = [Buf(), Buf()]
                src_ap, src_key = src; dst_ap, dst_key = dst
                npipe.part1(0); npipe.part2(0)
                ng_ = 0
                for g in range(NG):
                    hT = npipe.hT
                    for t in range(4):
                        tt = g * 4 + t
                        ysl = tt % 2
                        k.dma(sp, yin[ysl][:, 0:512], ya_d[:, tt, :], r=[k.dbuf(('ya', g))], w=[b_yin[ysl]])
                        k.dma(sp, yin[ysl][:, 512:1536], ybc_d[:, tt, :], r=[k.dbuf(('ybc', tt))], w=[b_yin[ysl]])
                        k.dma(sp, xr[ysl][:], src_ap[tt * 128:(tt + 1) * 128, :], r=[k.dbuf((src_key, tt))], w=[b_xr[ysl]])
                        t6 = ps[6][:].bitcast(BF16); t7 = ps[7][:].bitcast(BF16)
                        for c in range(12):
                            dstp = t6[:, c * 128:(c + 1) * 128] if c < 8 else t7[:, (c - 8) * 128:(c - 7) * 128]
                            bkk = 6 if c < 8 else 7
                            k.op(pe, lambda: T.transpose(dstp, yin[ysl][:, c * 128:(c + 1) * 128], identb[:]), r=[b_yin[ysl], b_id], w=[psb[bkk]],
                                 inc=(c == 7 or c == 11))
                        k.op(act, lambda: A.copy(out=yT[:, 0:8, :].rearrange("p a b -> p (a b)"), in_=t6), r=[psb[6]], w=[b_yT])
                        k.op(dve, lambda: V.tensor_copy(out=yT[:, 8:12, :].rearrange("p a b -> p (a b)"), in_=t7[:, 0:512]), r=[psb[7]], w=[b_yT])
                        for half in range(2):
                            for b in range(3):
                                gb = ng_ % 2; bb = 2 + ng_ % 2; ng_ += 1
                                for kk in range(8):
                                    mm(ps[gb][:, :], hT[:, kk, t * 128:(t + 1) * 128], WG[:, kk, b * 1024 + half * 512:b * 1024 + (half + 1) * 512],
                                       r=[npipe.b_hT, b_wg[b * 2 + half]], w=[psb[gb]], start=(kk == 0), stop=(kk == 7), inc=(kk == 7))
                                k.op(act, lambda: A.activation(out=sg[gb][:], in_=ps[gb][:, :], func=AF.Sigmoid), r=[psb[gb]], w=[b_sg[gb]])
                                for kk in range(4):
                                    mm(ps[bb][:, :], yT[:, b * 4 + kk, :], WBR[:, b, kk, half * 512:(half + 1) * 512], r=[b_yT, b_wbr[b]], w=[psb[bb]],
                                       start=(kk == 0), stop=(kk == 3), inc=(kk == 3))
                                if b == 0:
                                    k.op(dve, lambda: V.tensor_tensor(out=mg[:], in0=sg[gb][:], in1=ps[bb][:, :], op=ALU.mult), r=[b_sg[gb], psb[bb]],
                                         w=[b_mg])
                                else:
                                    k.op(dve, lambda: V.tensor_tensor(out=mt[:], in0=sg[gb][:], in1=ps[bb][:, :], op=ALU.mult), r=[b_sg[gb], psb[bb]],
                                         w=[b_mt])
                                    if b == 1:
                                        k.op(pool, lambda: G.tensor_tensor(out=mg[:], in0=mg[:], in1=mt[:], op=ALU.add), r=[b_mg, b_mt], w=[b_mg])
                                    else:
                                        k.op(pool, lambda: G.tensor_tensor(out=mb[:, half * 512:(half + 1) * 512], in0=mg[:], in1=mt[:], op=ALU.add),
                                             r=[b_mg, b_mt], w=[b_mb])
                        for c in range(8):
                            k.op(pe, lambda: T.transpose(t6[:, c * 128:(c + 1) * 128], mb[:, c * 128:(c + 1) * 128], identb[:]), r=[b_mb, b_id],
                                 w=[psb[6]], inc=(c == 7))
                        k.op(act, lambda: A.copy(out=mT[:].rearrange("p a b -> p (a b)"), in_=t6), r=[psb[6]], w=[b_mT])
                        for half in range(2):
                            for kk in range(8):
                                mm(ps[4][:, :], mT[:, kk, :], WO[:, kk, half * 512:(half + 1) * 512], r=[b_mT, b_wo], w=[psb[4]], start=(kk == 0),
                                   stop=(kk == 7), inc=(kk == 7))
                            k.op(dve, lambda: V.tensor_tensor(out=xr[ysl][:, half * 512:(half + 1) * 512], in0=ps[4][:, :],
                                                              in1=xr[ysl][:, half * 512:(half + 1) * 512], op=ALU.add), r=[psb[4], b_xr[ysl]],
                                 w=[b_xr[ysl]])
                        k.dma(sp, dst_ap[tt * 128:(tt + 1) * 128, :], xr[ysl][:], r=[b_xr[ysl]], w=[k.dbuf((dst_key, tt))])
                        if t == 1 and g + 1 < NG:
                            npipe.part1(g + 1)
                    if g + 1 < NG:
                        npipe.part2(g + 1)
                k.barrier()

        k.barrier()
        XR = (xres, 'xres')
        for l in range(DEPTH):
            if PH is None or 'ffn1' in PH: ffn_phase(l, 1, (x_in, 'x') if l == 0 else XR, XR, False)
            if PH is None or 'm1a' in PH: m1a_phase(l, XR)
            if PH is None or 'attn' in PH: attn_phase(l)
            if PH is None or 'm1b' in PH: m1b_phase(l, XR)
            if PH is None or 'm1c' in PH: m1c_phase(l, XR, XR)
            if PH is None or 'ffn2' in PH: ffn_phase(l, 2, XR, XR, l == DEPTH - 1)
        k.barrier()
    return nc


def make_consts(S):
    NT = S // 128
    c = {}
    c['c_ident'] = np.eye(128, dtype=np.float32)
    d = 64
    inv = (10000.0 ** (-np.arange(0, d, 2, dtype=np.float32) / np.float32(d))).astype(np.float32)
    ang = (np.arange(S, dtype=np.float32)[:, None] * inv[None, :]).astype(np.float32)
    cs = np.concatenate([np.cos(ang), np.sin(ang)], axis=1).astype(np.float32)
    c['c_rope'] = np.ascontiguousarray(cs.reshape(NT, 128, 64).transpose(1, 0, 2))
    h = np.arange(4, dtype=np.float64)
    log_g = np.log1p(-np.exp2(-5.0 - h))
    pos = np.arange(128, dtype=np.float64)
    eq = np.exp(log_g[None, :] * (pos[:, None] + 1.0))
    ek = np.exp(-log_g[None, :] * (pos[:, None] + 1.0)) * 0.125
    rt = np.concatenate([np.repeat(eq, 64, axis=1), np.repeat(ek, 64, axis=1)], axis=1)
    c['c_rtab'] = rt.astype(np.float32)
    j = np.arange(128)
    up = (j[:, None] <= j[None, :])
    c['c_tri'] = (up * (-1.0 / 16.0)).astype(np.float32)
    c['c_mask'] = up.astype(np.float32)
    rd = np.zeros((128, 2), np.float64)
    for pr in range(2):
        for hp in range(2):
            rd[hp * 64:(hp + 1) * 64, pr] = np.exp(log_g[2 * pr + hp] * 128.0)
    c['c_rdec'] = rd.astype(np.float32)
    return c


_CACHE = {}


def kernel(**inputs):
    S, DEPTH, B = 8192, 4, 4
    if 'nc' not in _CACHE:
        _CACHE['nc'] = build(S, DEPTH)
    nc = _CACHE['nc']
    consts = make_consts(S)
    x = np.ascontiguousarray(np.asarray(inputs['x'], dtype=np.float32))
    shared = {n: np.ascontiguousarray(np.asarray(inputs[n], dtype=np.float32)) for n, _ in WSPEC}
    shared['final_norm'] = np.ascontiguousarray(np.asarray(inputs['final_norm'], dtype=np.float32))
    shared.update(consts)
    in_maps = []
    for c in range(B):
        m = dict(shared); m['x'] = x[c]
        in_maps.append(m)
    res = run_bass_kernel_spmd(nc, in_maps, core_ids=list(range(B)))
    return np.stack([np.asarray(r['y']).reshape(S, D) for r in res.results], axis=0).astype(np.float32)
```

```python
import math, contextlib
import numpy as np
import concourse.bass as bass, concourse.mybir as mybir
from concourse.bass_utils import run_bass_kernel_spmd
from concourse.alu_op_type import AluOpType as ALU

F32, BF16 = mybir.dt.float32, mybir.dt.bfloat16
AF = mybir.ActivationFunctionType
D = 1024; DFF = 2752; NH = 4; IN_COLS = 7696; EPS = 1e-6
STRICT_SAME = True

WSPEC = [('ffn1_norm', (D,)), ('ffn1_w1', (D, DFF)), ('ffn1_w3', (D, DFF)), ('ffn1_w2', (DFF, D)),
         ('mix_norm', (D,)), ('w_in', (D, IN_COLS)), ('lam_qk', (4, 64)), ('da_norm', (128,)),
         ('ret_norm', (128,)), ('gla_a2', (16, 256)), ('gla_a_bias', (256,)), ('gla_norm', (128,)),
         ('w_branch_a', (512, D)), ('w_branch_b', (512, D)), ('w_branch_c', (512, D)), ('w_out', (D, D)),
         ('ffn2_norm', (D,)), ('ffn2_w1', (D, DFF)), ('ffn2_w3', (D, DFF)), ('ffn2_w2', (DFF, D))]


class Lane:
    def __init__(s, nc, es, name):
        s.sem = es.enter_context(nc.semaphore(name)); s.cnt = 0; s.name = name


class Eng(Lane):
    def __init__(s, nc, es, name, h, nlanes=0):
        super().__init__(nc, es, name)
        s.h = h; s.seen = {}
        s.lanes = [Lane(nc, es, f"{name}_l{i}") for i in range(nlanes)]; s.li = 0


class Buf:
    __slots__ = ('w', 'r')

    def __init__(s):
        s.w = None; s.r = {}


class K:
    def __init__(s, nc, es):
        s.nc = nc
        s.pe = Eng(nc, es, "pe", nc.tensor)
        s.act = Eng(nc, es, "act", nc.scalar)
        s.dve = Eng(nc, es, "dve", nc.vector)
        s.pool = Eng(nc, es, "pool", nc.gpsimd, nlanes=6)
        s.sp = Eng(nc, es, "sp", nc.sync, nlanes=10)
        s.engs = [s.pe, s.act, s.dve, s.pool, s.sp]
        s.dram = {}

    def dbuf(s, key):
        b = s.dram.get(key)
        if b is None:
            b = s.dram[key] = Buf()
        return b

    def _waits(s, eng, r, w, extra=()):
        need = {}

        def add(t):
            if t is None: return
            l, c = t
            if need.get(l, 0) < c: need[l] = c
        for b in r: add(b.w)
        for b in w:
            add(b.w)
            for l, c in b.r.items(): add((l, c))
        for t in extra: add(t)
        for l, c in need.items():
            if l is eng and (not STRICT_SAME or c > eng.cnt): continue
            if eng.seen.get(l, 0) >= c: continue
            eng.h.wait_ge(l.sem, c); eng.seen[l] = c

    def op(s, eng, fn, r=(), w=(), inc=True):
        s._waits(eng, r, w)
        inst = fn()
        tgt = eng.cnt + 1
        if inc:
            inst.then_inc(eng.sem, 1); eng.cnt = tgt
        for b in r: b.r[eng] = tgt
        for b in w:
            b.w = (eng, tgt); b.r = {}
        return inst

    def dma(s, eng, out, in_, r=(), w=(), **kw):
        lane = eng.lanes[eng.li]; eng.li = (eng.li + 1) % len(eng.lanes)
        s._waits(eng, r, w, extra=[(lane, lane.cnt)] if lane.cnt else ())
        eng.h.dma_start(out=out, in_=in_, **kw).then_inc(lane.sem, 16)
        lane.cnt += 16
        for b in r: b.r[lane] = lane.cnt
        for b in w:
            b.w = (lane, lane.cnt); b.r = {}

    def barrier(s):
        alll = []
        for e in s.engs:
            alll.append(e); alll.extend(e.lanes)
        for e in s.engs:
            for l in alll:
                if l is e or l.cnt == 0: continue
                if e.seen.get(l, 0) >= l.cnt: continue
                e.h.wait_ge(l.sem, l.cnt); e.seen[l] = l.cnt


def lam_init_of(l):
    return 0.8 - 0.6 * math.exp(-0.3 * l)


def build(S=8192, DEPTH=4, PH=None):
    NT = S // 128; NG = S // 512
    nc = bass.Bass("TRN2", target_bir_lowering=False)

    def dr(n, sh, dt=F32, kind="ExternalInput"):
        return nc.dram_tensor(n, list(sh), dt, kind=kind).ap()
    x_in = dr("x", [S, D])
    W = {n: dr(n, (DEPTH,) + sh) for n, sh in WSPEC}
    fin_g = dr("final_norm", [D])
    c_ident = dr("c_ident", [128, 128])
    c_rope = dr("c_rope", [128, NT, 64])
    c_rtab = dr("c_rtab", [128, 512])
    c_tri = dr("c_tri", [128, 128])
    c_mask = dr("c_mask", [128, 128])
    c_rdec = dr("c_rdec", [128, 2])
    y_out = dr("y", [S, D], kind="ExternalOutput")
    xres = dr("xres", [S, D], kind="Internal")
    qT_d = dr("qT_d", [4, 128, S], BF16, kind="Internal")
    kT_d = dr("kT_d", [4, 128, S], BF16, kind="Internal")
    v_d = dr("v_d", [128, NT, 4, 129], BF16, kind="Internal")
    ya_d = dr("ya_d", [128, NT, 512], BF16, kind="Internal")
    ybc_d = dr("ybc_d", [128, NT, 1024], BF16, kind="Internal")

    es = contextlib.ExitStack()
    with es:
        k = K(nc, es)
        pe, act, dve, pool, sp = k.pe, k.act, k.dve, k.pool, k.sp
        T, V, A, G = nc.tensor, nc.vector, nc.scalar, nc.gpsimd

        uid = [0]

        def sbt(stack, n, sh, dt):
            uid[0] += 1
            return stack.enter_context(nc.sbuf_tensor(f"{n}_{uid[0]}", list(sh), dt))
        psall = es.enter_context(nc.psum_tensor("psall", [128, 4096], F32))
        ps = [psall[:, i * 512:(i + 1) * 512] for i in range(8)]
        psb = [Buf() for _ in range(8)]
        identb = sbt(es, "identb", [128, 128], BF16); b_id = Buf()
        mh = sbt(es, "mh", [128, 1], F32); b_mh = Buf()
        k.dma(pool, identb[:], c_ident[:], w=[b_id])
        mh4 = sbt(es, "mh4", [128, 4], F32)
        k.op(pool, lambda: G.memset(mh[:], -0.5), w=[b_mh])
        k.op(pool, lambda: G.memset(mh4[:], -0.5), w=[b_mh])

        def mm(out, lhsT, rhs, r, w, start=True, stop=True, inc=True, **kw):
            return k.op(pe, lambda: T.matmul(out, lhsT, rhs, start=start, stop=stop, **kw), r=r, w=w, inc=inc)

        class NormPipe:
            def __init__(s, st, src, gain_ap, pst_banks):
                s.src = src
                s.xn = [sbt(st, f"xn{i}", [128, D], F32) for i in range(2)]; s.b_xn = [Buf(), Buf()]
                s.hb = [sbt(st, f"hb{i}", [128, D], BF16) for i in range(4)]; s.b_hb = [Buf() for _ in range(4)]
                s.hT = sbt(st, "hT", [128, 8, 512], BF16); s.b_hT = Buf()
                s.gB = sbt(st, "gB", [128, D], F32); s.b_gB = Buf()
                s.junk = sbt(st, "junk", [128, D], BF16); s.b_junk = Buf()
                s.ss = sbt(st, "nss", [128, 16], F32); s.b_ss = [Buf() for _ in range(4)]
                s.pst = pst_banks; s.n = 0
                k.dma(sp, s.gB[:], gain_ap.partition_broadcast(128), w=[s.b_gB])

            def part1(s, g):
                src_ap, src_key = s.src
                for t in range(4):
                    tt = g * 4 + t
                    xn = s.xn[t % 2]; bx = s.b_xn[t % 2]; ss = s.ss; bs = s.b_ss[t]
                    k.dma(sp, xn[:], src_ap[tt * 128:(tt + 1) * 128, :], r=[k.dbuf((src_key, tt))], w=[bx])
                    k.op(act, lambda: A.activation(out=s.junk[:], in_=xn[:], func=AF.Square, accum_out=ss[:, 4 * t:4 * t + 1]),
                         r=[bx], w=[s.b_junk, bs])
                    k.op(dve, lambda: V.tensor_scalar(out=ss[:, 4 * t + 1:4 * t + 2], in0=ss[:, 4 * t:4 * t + 1], scalar1=1.0 / D,
                                                      scalar2=EPS, op0=ALU.mult, op1=ALU.add), r=[bs], w=[bs])
                    k.op(pool, lambda: G.tensor_tensor(out=ss[:, 4 * t + 2:4 * t + 3], in0=ss[:, 4 * t + 1:4 * t + 2], in1=mh[:, 0:1],
                                                       op=ALU.pow), r=[bs, b_mh], w=[bs])
                    k.op(dve, lambda: V.scalar_tensor_tensor(out=s.hb[t][:], in0=xn[:], scalar=ss[:, 4 * t + 2:4 * t + 3], in1=s.gB[:],
                                                             op0=ALU.mult, op1=ALU.mult), r=[bx, bs, s.b_gB], w=[s.b_hb[t]])

            def part2(s, g):
                for t in range(4):
                    bank = s.pst[s.n % len(s.pst)]; s.n += 1
                    pst = ps[bank][:].bitcast(BF16)
                    for c in range(8):
                        k.op(pe, lambda: T.transpose(pst[:, c * 128:(c + 1) * 128], s.hb[t][:, c * 128:(c + 1) * 128], identb[:]),
                             r=[s.b_hb[t], b_id], w=[psb[bank]], inc=(c == 7))
                    src = pst.rearrange("p (c t) -> p c t", c=8)
                    if t % 2 == 0:
                        k.op(dve, lambda: V.tensor_copy(out=s.hT[:, :, t * 128:(t + 1) * 128], in_=src), r=[psb[bank]], w=[s.b_hT])
                    else:
                        k.op(act, lambda: A.copy(out=s.hT[:, :, t * 128:(t + 1) * 128], in_=src), r=[psb[bank]], w=[s.b_hT])

        def wload(dst, src_rows_fn, nk, ncols, piece, bufs):
            npieces = (ncols + piece - 1) // piece
            for pi in range(npieces):
                c0 = pi * piece; c1 = min(ncols, c0 + piece)
                for c in range(nk):
                    src = src_rows_fn(c)
                    rows = src.shape[0]
                    k.dma(pool, dst[0:rows, c, c0:c1], src[:, c0:c1], w=[bufs[pi]])

        def ffn_phase(l, which, src, dst, final):
            pre = 'ffn1' if which == 1 else 'ffn2'
            with contextlib.ExitStack() as st:
                WS = sbt(st, "WS", [128, 8 * DFF * 2 + 22 * D], BF16)
                W1 = WS[:, 0:8 * DFF].rearrange("p (c f) -> p c f", c=8)
                W3 = WS[:, 8 * DFF:16 * DFF].rearrange("p (c f) -> p c f", c=8)
                W2 = WS[:, 16 * DFF:16 * DFF + 22 * D].rearrange("p (c f) -> p c f", c=22)
                PIECE = 1408
                b_w1 = [Buf(), Buf()]; b_w3 = [Buf(), Buf()]; b_w2 = [Buf()]
                w1d, w3d, w2d = W[pre + '_w1'], W[pre + '_w3'], W[pre + '_w2']
                for pi in range(2):
                    c0 = pi * PIECE; c1 = min(DFF, c0 + PIECE)
                    for (wd, Wt, bb) in ((w1d, W1, b_w1), (w3d, W3, b_w3)):
                        for c in range(8):
                            k.dma(pool, Wt[:, c, c0:c1], wd[l, c * 128:(c + 1) * 128, c0:c1], w=[bb[pi]])
                for c in range(22):
                    rows = 128 if c < 21 else 64
                    k.dma(pool, W2[0:rows, c, :], w2d[l, c * 128:c * 128 + rows, :], w=[b_w2[0]])
                npipe = NormPipe(st, src, W[pre + '_norm'][l, :], [6, 7])
                u = sbt(st, "u", [128, 22, 512], BF16); b_u = [Buf() for _ in range(22)]
                sa = [sbt(st, f"sa{i}", [128, 512], F32) for i in range(2)]; b_sa = [Buf(), Buf()]
                xr = [sbt(st, f"xr{i}", [128, D], F32) for i in range(2)]; b_xr = [Buf(), Buf()]
                if final:
                    gF = sbt(st, "gF", [128, D], F32); b_gF = Buf()
                    k.dma(sp, gF[:], fin_g.partition_broadcast(128), w=[b_gF])
                    fss = sbt(st, "fss", [128, 8], F32); b_fss = [Buf(), Buf()]
                src_ap, src_key = src
                dst_ap, dst_key = dst
                npipe.part1(0); npipe.part2(0)
                nxr = 0
                for g in range(NG):
                    for f in range(22):
                        fs = 128 if f < 21 else 64
                        pa = f % 2; pb = 2 + f % 2
                        pi = 0 if f * 128 < PIECE else 1
                        for kk in range(8):
                            mm(ps[pa][0:fs, :], W1[:, kk, f * 128:f * 128 + fs], npipe.hT[:, kk, :], r=[npipe.b_hT, b_w1[pi]],
                               w=[psb[pa]], start=(kk == 0), stop=(kk == 7), inc=(kk == 7))
                        for kk in range(8):
                            mm(ps[pb][0:fs, :], W3[:, kk, f * 128:f * 128 + fs], npipe.hT[:, kk, :], r=[npipe.b_hT, b_w3[pi]],
                               w=[psb[pb]], start=(kk == 0), stop=(kk == 7), inc=(kk == 7))
                        k.op(act, lambda: A.activation(out=sa[f % 2][0:fs, :], in_=ps[pa][0:fs, :], func=AF.Silu),
                             r=[psb[pa]], w=[b_sa[f % 2]])
                        k.op(dve, lambda: V.tensor_tensor(out=u[0:fs, f, :], in0=sa[f % 2][0:fs, :], in1=ps[pb][0:fs, :], op=ALU.mult),
                             r=[b_sa[f % 2], psb[pb]], w=[b_u[f]])
                        if f == 8 and g + 1 < NG:
                            npipe.part1(g + 1)
                    if g + 1 < NG:
                        npipe.part2(g + 1)
                    for t in range(4):
                        tt = g * 4 + t
                        xs = nxr % 2; nxr += 1
                        k.dma(sp, xr[xs][:], src_ap[tt * 128:(tt + 1) * 128, :], r=[k.dbuf((src_key, tt))], w=[b_xr[xs]])
                        for half in range(2):
                            py = 4 + half
                            for f in range(22):
                                fs = 128 if f < 21 else 64
                                mm(ps[py][:, :], u[0:fs, f, t * 128:(t + 1) * 128], W2[0:fs, f, half * 512:(half + 1) * 512],
                                   r=[b_u[f], b_w2[0]], w=[psb[py]], start=(f == 0), stop=(f == 21), inc=(f == 21))
                            k.op(dve, lambda: V.scalar_tensor_tensor(out=xr[xs][:, half * 512:(half + 1) * 512], in0=ps[py][:, :], scalar=0.5,
                                                                     in1=xr[xs][:, half * 512:(half + 1) * 512], op0=ALU.mult, op1=ALU.add),
                                 r=[psb[py], b_xr[xs]], w=[b_xr[xs]])
                        if not final:
                            k.dma(sp, dst_ap[tt * 128:(tt + 1) * 128, :], xr[xs][:], r=[b_xr[xs]], w=[k.dbuf((dst_key, tt))])
                        else:
                            k.op(act, lambda: A.activation(out=npipe.junk[:], in_=xr[xs][:], func=AF.Square, accum_out=fss[:, 4 * xs:4 * xs + 1]),
                                 r=[b_xr[xs]], w=[npipe.b_junk, b_fss[xs]])
                            k.op(dve, lambda: V.tensor_scalar(out=fss[:, 4 * xs + 1:4 * xs + 2], in0=fss[:, 4 * xs:4 * xs + 1], scalar1=1.0 / D,
                                                              scalar2=EPS, op0=ALU.mult, op1=ALU.add), r=[b_fss[xs]], w=[b_fss[xs]])
                            k.op(pool, lambda: G.tensor_tensor(out=fss[:, 4 * xs + 2:4 * xs + 3], in0=fss[:, 4 * xs + 1:4 * xs + 2],
                                                               in1=mh[:, 0:1], op=ALU.pow), r=[b_fss[xs], b_mh], w=[b_fss[xs]])
                            k.op(dve, lambda: V.scalar_tensor_tensor(out=xr[xs][:], in0=xr[xs][:], scalar=fss[:, 4 * xs + 2:4 * xs + 3],
                                                                     in1=gF[:], op0=ALU.mult, op1=ALU.mult),
                                 r=[b_xr[xs], b_fss[xs], b_gF], w=[b_xr[xs]])
                            k.dma(sp, y_out[tt * 128:(tt + 1) * 128, :], xr[xs][:], r=[b_xr[xs]], w=[k.dbuf(('y', tt))])
                k.barrier()

        def m1a_phase(l, src):
            with contextlib.ExitStack() as st:
                WA = sbt(st, "WA", [128, 8, 1536], BF16); b_wa = [Buf(), Buf(), Buf()]
                wload(WA, lambda c: W['w_in'][l, c * 128:(c + 1) * 128, 0:1536], 8, 1536, 512, b_wa)
                npipe = NormPipe(st, src, W['mix_norm'][l, :], [6, 7])
                qk = [sbt(st, f"qk{i}", [128, 512], BF16) for i in range(2)]; b_qk = [Buf(), Buf()]
                vs = [sbt(st, f"vs{i}", [128, 4, 4, 129], BF16) for i in range(2)]; b_vs = [Buf(), Buf()]
                for i in range(2):
                    k.op(pool, lambda: G.memset(vs[i][:], 1.0), w=[b_vs[i]])
                npipe.part1(0); npipe.part2(0)
                for g in range(NG):
                    for c in range(8):
                        pb_ = c % 2
                        for kk in range(8):
                            mm(ps[pb_][:, :], WA[:, kk, c * 128:(c + 1) * 128], npipe.hT[:, kk, :], r=[npipe.b_hT, b_wa[c // 4]],
                               w=[psb[pb_]], start=(kk == 0), stop=(kk == 7), inc=(kk == 7))
                        if c % 2 == 0:
                            k.op(act, lambda: A.copy(out=qk[pb_][:], in_=ps[pb_][:, :]), r=[psb[pb_]], w=[b_qk[pb_]])
                        else:
                            k.op(dve, lambda: V.tensor_copy(out=qk[pb_][:], in_=ps[pb_][:, :]), r=[psb[pb_]], w=[b_qk[pb_]])
                        dd, key = (qT_d, 'qT') if c < 4 else (kT_d, 'kT')
                        k.dma(sp, dd[c % 4, :, g * 512:(g + 1) * 512], qk[pb_][:], r=[b_qk[pb_]], w=[k.dbuf((key, c % 4, g))])
                        if c == 3 and g + 1 < NG:
                            npipe.part1(g + 1)
                    vsl = g % 2
                    for t in range(4):
                        pv = 2 + t % 2
                        for kk in range(8):
                            mm(ps[pv][:, :], npipe.hT[:, kk, t * 128:(t + 1) * 128], WA[:, kk, 1024:1536], r=[npipe.b_hT, b_wa[2]],
                               w=[psb[pv]], start=(kk == 0), stop=(kk == 7), inc=(kk == 7))
                        src_v = ps[pv][:, :].rearrange("p (h e) -> p h e", h=4)
                        if t % 2 == 0:
                            k.op(act, lambda: A.copy(out=vs[vsl][:, t, :, 0:128], in_=src_v), r=[psb[pv]], w=[b_vs[vsl]])
                        else:
                            k.op(dve, lambda: V.tensor_copy(out=vs[vsl][:, t, :, 0:128], in_=src_v), r=[psb[pv]], w=[b_vs[vsl]])
                    k.dma(sp, v_d[:, g * 4:(g + 1) * 4, :, :], vs[vsl][:], r=[b_vs[vsl]], w=[k.dbuf(('v', g))])
                    if g + 1 < NG:
                        npipe.part2(g + 1)
                k.barrier()

        def attn_phase(l):
            li = lam_init_of(l)
            with contextlib.ExitStack() as st:
                kT = sbt(st, "kT", [128, 4, S], BF16); b_kT = [Buf() for _ in range(4)]
                va = sbt(st, "va", [128, NT, 4, 129], BF16); b_va = Buf()
                qg = [sbt(st, f"qg{i}", [128, 4, 512], BF16) for i in range(2)]; b_qg = [Buf(), Buf()]
                Pt = [sbt(st, f"Pt{i}", [128, 512], BF16) for i in range(4)]; b_P = [Buf() for _ in range(4)]
                yag = [sbt(st, f"yag{i}", [128, 4, 512], BF16) for i in range(2)]; b_yag = [Buf(), Buf()]
                lq = sbt(st, "lq", [128, 256], F32); b_lq = Buf()
                lt = sbt(st, "lt", [128, 8], F32); b_lt = Buf()
                ljunk = sbt(st, "ljunk", [128, 64], F32); b_lj = Buf()
                gda = sbt(st, "gda", [128, 128], F32); b_gda = Buf()
                sm = sbt(st, "sm", [128, 12], F32); b_sm = Buf()
                sm2 = sbt(st, "sm2", [128, 12], F32); b_sm2 = Buf()
                tmp = sbt(st, "atmp", [128, 128], F32); b_tmp = Buf()
                o = sbt(st, "ao", [128, 128], F32); b_o = Buf()
                oj = sbt(st, "aoj", [128, 128], BF16); b_oj = Buf()
                for hd in range(4):
                    k.dma(sp, kT[:, hd, :], kT_d[hd, :, :], r=[k.dbuf(('kT', hd, g)) for g in range(NG)], w=[b_kT[hd]])
                k.dma(sp, va[:], v_d[:], r=[k.dbuf(('v', g)) for g in range(NG)], w=[b_va])
                k.dma(sp, lq[:], W['lam_qk'][l].rearrange("a b -> (a b)").partition_broadcast(128), w=[b_lq])
                k.dma(sp, gda[:], W['da_norm'][l, :].partition_broadcast(128), w=[b_gda])
                for i_ in range(2):
                    k.op(dve, lambda: V.tensor_tensor(out=ljunk[:], in0=lq[:, i_ * 128:i_ * 128 + 64], in1=lq[:, i_ * 128 + 64:i_ * 128 + 128],
                                                      op=ALU.mult), r=[b_lq], w=[b_lj])
                    k.op(act, lambda: A.activation(out=ljunk[:], in_=ljunk[:], func=AF.Copy, accum_out=lt[:, i_:i_ + 1]), r=[b_lj], w=[b_lj, b_lt])
                k.op(act, lambda: A.activation(out=lt[:, 2:4], in_=lt[:, 0:2], func=AF.Exp), r=[b_lt], w=[b_lt])
                k.op(dve, lambda: V.tensor_tensor(out=lt[:, 4:5], in0=lt[:, 3:4], in1=lt[:, 2:3], op=ALU.subtract), r=[b_lt], w=[b_lt])
                k.op(dve, lambda: V.tensor_scalar(out=lt[:, 5:6], in0=lt[:, 4:5], scalar1=-li, scalar2=None, op0=ALU.add), r=[b_lt], w=[b_lt])
                k.op(dve, lambda: V.tensor_scalar(out=gda[:], in0=gda[:], scalar1=(1.0 - li), scalar2=None, op0=ALU.mult), r=[b_gda], w=[b_gda])
                accS = [sbt(st, f"accS{i}", [128, 2, 2, 258], F32) for i in range(2)]; b_accS = [Buf(), Buf()]
                o4 = sbt(st, "o4", [128, 4, 128], F32); b_o4 = Buf()
                sq = sbt(st, "sq", [128, 128], F32); b_sq = Buf()
                P3 = [sbt(st, f"P3_{i}", [128, 2, 512], BF16) for i in range(3)]; b_P3 = [Buf() for _ in range(3)]
                blocks = [(Gq, hd, kb) for Gq in range(NG) for hd in range(4) for kb in range(4 * (Gq + 1))]
                nfin = [0]

                def s1(i):
                    Gq, hd, kb = blocks[i]
                    qs = Gq % 2
                    if hd == 0 and kb == 0:
                        k.dma(sp, qg[qs][:], qT_d[:, :, Gq * 512:(Gq + 1) * 512].rearrange("h p s -> p h s"),
                              r=[k.dbuf(('qT', h_, Gq)) for h_ in range(4)], w=[b_qg[qs]])
                    dg = kb - 4 * Gq; t0 = max(dg, 0); q0 = t0 * 128
                    b0 = 4 + 2 * (i % 2)
                    for m in range(2):
                        mm(ps[b0 + m][:, q0:512], kT[m * 64:(m + 1) * 64, hd, kb * 128:(kb + 1) * 128],
                           qg[qs][m * 64:(m + 1) * 64, hd, q0:512], r=[b_kT[hd], b_qg[qs]], w=[psb[b0 + m]])
                    pslot = i % 3
                    src = psall[:, b0 * 512:(b0 + 2) * 512].rearrange("p (m q) -> p m q", m=2)[:, :, q0:512]
                    k.op(act, lambda: A.activation(out=P3[pslot][:, :, q0:512], in_=src, func=AF.Exp, scale=0.125),
                         r=[psb[b0], psb[b0 + 1]], w=[b_P3[pslot]])
                    if dg >= 0:
                        k.op(pool, lambda: G.memset(P3[pslot][64:128, :, q0:q0 + 64], 0.0), w=[b_P3[pslot]])

                def s2(i):
                    Gq, hd, kb = blocks[i]
                    dg = kb - 4 * Gq; t0 = max(dg, 0)
                    pslot = i % 3
                    for m in range(2):
                        for t in range(t0, 4):
                            ab = m * 2 + t // 2
                            acc = ps[ab][:, (t % 2) * 129:(t % 2) * 129 + 129]
                            mm(acc, P3[pslot][:, m, t * 128:(t + 1) * 128], va[:, kb, hd, :], r=[b_P3[pslot], b_va], w=[psb[ab]],
                               start=(kb == 0 and t % 2 == 0), stop=(kb == 4 * Gq + t), inc=(m == 1 and t == 3), skip_group_check=True)
                    if kb == 4 * Gq + 3:
                        finalize(Gq, hd)

                def finalize(Gq, hd):
                    ys = Gq % 2
                    asl = nfin[0] % 2; nfin[0] += 1
                    aS = accS[asl]; bA = b_accS[asl]
                    for m in range(2):
                        src = psall[:, (2 * m) * 512:(2 * m + 2) * 512].rearrange("p (b q) -> p b q", b=2)[:, :, 0:258]
                        k.op(dve, lambda: V.tensor_copy(out=aS[:, m, :, :], in_=src), r=[psb[2 * m], psb[2 * m + 1]], w=[bA])
                    a5 = aS[:].rearrange("p m b (t e) -> p m b t e", t=2)
                    lcol = a5[:, :, :, :, 128:129].rearrange("p m b t e -> p (m b t e)")
                    k.op(dve, lambda: V.reciprocal(out=sm[:, 0:8], in_=lcol), r=[bA], w=[b_sm])
                    k.op(dve, lambda: V.tensor_scalar(out=sm[:, 8:12], in0=sm[:, 4:8], scalar1=lt[:, 5:6], scalar2=None, op0=ALU.mult),
                         r=[b_sm, b_lt], w=[b_sm])
                    for t in range(4):
                        a0 = a5[:, 0, t // 2, t % 2, 0:128]; a1 = a5[:, 1, t // 2, t % 2, 0:128]
                        k.op(dve, lambda: V.tensor_scalar(out=tmp[:], in0=a1, scalar1=sm[:, 8 + t:9 + t], scalar2=None, op0=ALU.mult),
                             r=[bA, b_sm], w=[b_tmp])
                        k.op(dve, lambda: V.scalar_tensor_tensor(out=o4[:, t, :], in0=a0, scalar=sm[:, t:t + 1], in1=tmp[:], op0=ALU.mult,
                                                                 op1=ALU.add), r=[bA, b_sm, b_tmp], w=[b_o4])
                        k.op(dve, lambda: V.scalar_tensor_tensor(out=sq[:], in0=o4[:, t, :], scalar=1.0, in1=o4[:, t, :], op0=ALU.mult,
                                                                 op1=ALU.mult, accum_out=sm2[:, t:t + 1]), r=[b_o4], w=[b_sq, b_sm2])
                    k.op(dve, lambda: V.tensor_scalar(out=sm2[:, 4:8], in0=sm2[:, 0:4], scalar1=1.0 / 128, scalar2=EPS, op0=ALU.mult,
                                                      op1=ALU.add), r=[b_sm2], w=[b_sm2])
                    k.op(pool, lambda: G.tensor_tensor(out=sm2[:, 8:12], in0=sm2[:, 4:8], in1=mh4[:, 0:4], op=ALU.pow), r=[b_sm2, b_mh], w=[b_sm2])
                    for t in range(4):
                        k.op(dve, lambda: V.scalar_tensor_tensor(out=yag[ys][:, t, hd * 128:(hd + 1) * 128], in0=o4[:, t, :],
                                                                 scalar=sm2[:, 8 + t:9 + t], in1=gda[:], op0=ALU.mult, op1=ALU.mult),
                             r=[b_o4, b_sm2, b_gda], w=[b_yag[ys]])
                    if hd == 3:
                        k.dma(sp, ya_d[:, Gq * 4:(Gq + 1) * 4, :], yag[ys][:], r=[b_yag[ys]], w=[k.dbuf(('ya', Gq))])

                LA = 2
                for i in range(len(blocks) + LA):
                    if i < len(blocks): s1(i)
                    if i >= LA: s2(i - LA)
                k.barrier()

        def m1b_phase(l, src):
            NC_B = 3088
            with contextlib.ExitStack() as st:
                WB = sbt(st, "WB", [128, 8, NC_B], BF16); b_wb = [Buf() for _ in range(7)]
                offs = [0, 512, 1024, 1536, 2048, 2560, 3072, 3088]
                for pi in range(7):
                    for c in range(8):
                        k.dma(pool, WB[:, c, offs[pi]:offs[pi + 1]], W['w_in'][l, c * 128:(c + 1) * 128, 1536 + offs[pi]:1536 + offs[pi + 1]],
                              w=[b_wb[pi]])
                npipe = NormPipe(st, src, W['mix_norm'][l, :], [3])
                rope = sbt(st, "rope", [128, NT, 64], F32); b_rope = Buf()
                rtab = sbt(st, "rtab", [128, 512], F32); b_rtab = Buf()
                tri = sbt(st, "tri", [128, 128], F32); maskb = sbt(st, "maskb", [128, 128], F32); b_cst = Buf()
                rdec = sbt(st, "rdec", [128, 2], F32)
                a2a = sbt(st, "a2a", [32, 256], F32); b_a2 = Buf()
                gn = sbt(st, "gn", [128, 2, 128], F32); b_gn = Buf()
                k.dma(sp, rope[:], c_rope[:], w=[b_rope])
                k.dma(sp, rtab[:], c_rtab[:], w=[b_rtab])
                k.dma(sp, tri[:], c_tri[:], w=[b_cst]); k.dma(sp, maskb[:], c_mask[:], w=[b_cst]); k.dma(sp, rdec[:], c_rdec[:], w=[b_cst])
                k.op(pool, lambda: G.memset(a2a[:], 0.0), w=[b_a2])
                k.dma(sp, a2a[0:16, :], W['gla_a2'][l], w=[b_a2])
                k.dma(sp, a2a[16:17, :], W['gla_a_bias'][l:l + 1, :], w=[b_a2])
                k.dma(sp, gn[:, 0, :], W['ret_norm'][l, :].partition_broadcast(128), w=[b_gn])
                k.dma(sp, gn[:, 1, :], W['gla_norm'][l, :].partition_broadcast(128), w=[b_gn])
                caT = sbt(st, "caT", [32, 512], F32); b_caT = Buf()
                vb = sbt(st, "vb", [128, 2, 512], BF16); b_vb = [Buf(), Buf()]
                GG = sbt(st, "GG", [128, 2, 512], F32); b_GG = [Buf(), Buf()]
                rt = sbt(st, "rt", [128, 6, 256], F32); b_rt = Buf()
                qkr = sbt(st, "qkr", [128, 512], F32); b_qkr = Buf()
                qkg = sbt(st, "qkg", [128, 2, 512], BF16); b_qkg = [Buf(), Buf()]
                spz = sbt(st, "spz", [128, 256], F32); b_spz = Buf()
                E12 = sbt(st, "E12", [128, 512], F32); b_E = Buf()
                dec = sbt(st, "dec", [128, 2], F32); b_dec = Buf()
                qkT = sbt(st, "qkT", [128, 2, 4, 128], BF16); b_qkT = [Buf(), Buf()]
                scm = [sbt(st, f"scm{i}", [128, 128], BF16) for i in range(2)]; b_scm = [Buf(), Buf()]
                stf = sbt(st, "stf", [128, 4, 128], F32); b_stf = [Buf() for _ in range(4)]
                stb = sbt(st, "stb", [128, 4, 128], BF16); b_stb = [Buf() for _ in range(4)]
                oss = sbt(st, "oss", [128, 24], F32); b_oss = Buf()
                ojk = sbt(st, "ojk", [128, 128], BF16); b_ojk = Buf()
                ybc = [sbt(st, f"ybc{i}", [128, 1024], BF16) for i in range(2)]; b_ybc = [Buf(), Buf()]
                for i in range(4):
                    k.op(pool, lambda: G.memset(stf[:, i, :], 0.0), w=[b_stf[i]])
                    k.op(pool, lambda: G.memset(stb[:, i, :], 0.0), w=[b_stb[i]])
                k.op(pool, lambda: G.memset(caT[:], 1.0), w=[b_caT])
                npipe.part1(0); npipe.part2(0)
                npj = 0
                for g in range(NG):
                    hT = npipe.hT
                    for kk in range(8):
                        mm(ps[2][0:16, :], WB[:, kk, 3072:3088], hT[:, kk, :], r=[npipe.b_hT, b_wb[6]], w=[psb[2]], start=(kk == 0), stop=(kk == 7),
                           inc=(kk == 7))
                    k.op(dve, lambda: V.tensor_copy(out=caT[0:16, :], in_=ps[2][0:16, :]), r=[psb[2]], w=[b_caT])
                    for t in range(4):
                        tt = g * 4 + t
                        lhs = lambda kk: hT[:, kk, t * 128:(t + 1) * 128]

                        def proj(pi):
                            nonlocal npj
                            bank = npj % 2; npj += 1
                            for kk in range(8):
                                mm(ps[bank][:, :], lhs(kk), WB[:, kk, offs[pi]:offs[pi + 1]], r=[npipe.b_hT, b_wb[pi]], w=[psb[bank]],
                                   start=(kk == 0), stop=(kk == 7), inc=(kk == 7))
                            return bank
                        for mx, pi in ((0, 1), (1, 4)):
                            bk = proj(pi)
                            k.op(act, lambda: A.copy(out=vb[:, mx, :], in_=ps[bk][:, :]), r=[psb[bk]], w=[b_vb[mx]])
                        for mx, pi in ((0, 2), (1, 5)):
                            bk = proj(pi)
                            k.op(act, lambda: A.activation(out=GG[:, mx, :], in_=ps[bk][:, :], func=AF.Silu), r=[psb[bk]], w=[b_GG[mx]])
                            k.op(pool, lambda: G.tensor_tensor(out=GG[:, mx, :].rearrange("p (h e) -> p h e", h=4),
                                                               in0=GG[:, mx, :].rearrange("p (h e) -> p h e", h=4),
                                                               in1=gn[:, mx, :].unsqueeze(1).broadcast_to([128, 4, 128]), op=ALU.mult),
                                 r=[b_GG[mx], b_gn], w=[b_GG[mx]])
                        bk = proj(0)
                        p3 = ps[bk][:, :].rearrange("p (a two d) -> p a two d", a=8, two=2)
                        cosb = rope[:, tt, 0:32].unsqueeze(1).broadcast_to([128, 8, 32])
                        sinb = rope[:, tt, 32:64].unsqueeze(1).broadcast_to([128, 8, 32])
                        r3 = lambda i: rt[:, i, :].rearrange("p (a d) -> p a d", a=8)
                        q3 = qkr[:].rearrange("p (a two d) -> p a two d", a=8, two=2)
                        rr_ = [psb[bk], b_rope]
                        k.op(dve, lambda: V.tensor_tensor(out=r3(0), in0=p3[:, :, 0, :], in1=cosb, op=ALU.mult), r=rr_, w=[b_rt])
                        k.op(dve, lambda: V.tensor_tensor(out=r3(1), in0=p3[:, :, 1, :], in1=sinb, op=ALU.mult), r=rr_, w=[b_rt])
                        k.op(dve, lambda: V.tensor_tensor(out=r3(2), in0=p3[:, :, 0, :], in1=sinb, op=ALU.mult), r=rr_, w=[b_rt])
                        k.op(dve, lambda: V.tensor_tensor(out=r3(3), in0=p3[:, :, 1, :], in1=cosb, op=ALU.mult), r=rr_, w=[b_rt])
                        k.op(dve, lambda: V.tensor_tensor(out=q3[:, :, 0, :], in0=r3(0), in1=r3(1), op=ALU.subtract), r=[b_rt], w=[b_qkr])
                        k.op(dve, lambda: V.tensor_tensor(out=q3[:, :, 1, :], in0=r3(2), in1=r3(3), op=ALU.add), r=[b_rt], w=[b_qkr])
                        k.op(dve, lambda: V.tensor_tensor(out=qkg[:, 0, :], in0=qkr[:], in1=rtab[:], op=ALU.mult), r=[b_qkr, b_rtab], w=[b_qkg[0]])
                        mm(ps[2][:, 0:256], caT[0:32, t * 128:(t + 1) * 128], a2a[0:32, :], r=[b_caT, b_a2], w=[psb[2]])
                        k.op(act, lambda: A.activation(out=spz[:], in_=ps[2][:, 0:256], func=AF.Exp, scale=-1.0), r=[psb[2]], w=[b_spz])
                        k.op(act, lambda: A.activation(out=spz[:], in_=spz[:], func=AF.Ln, bias=1.0), r=[b_spz], w=[b_spz])
                        mm(ps[2][:, 256:512], tri[:], spz[:], r=[b_cst, b_spz], w=[psb[2]])
                        for pr in range(2):
                            mm(ps[2][:, 2 * pr:2 * pr + 2], spz[:, pr * 128:(pr + 1) * 128], tri[:, 126:128], r=[b_cst, b_spz], w=[psb[2]],
                               inc=(pr == 1))
                        k.op(act, lambda: A.activation(out=E12[:, 0:256], in_=ps[2][:, 256:512], func=AF.Exp, bias=math.log(0.125)),
                             r=[psb[2]], w=[b_E])
                        k.op(act, lambda: A.activation(out=E12[:, 256:512], in_=ps[2][:, 256:512], func=AF.Exp, scale=-1.0), r=[psb[2]], w=[b_E])
                        k.op(act, lambda: A.activation(out=dec[:, 0:1], in_=ps[2][:, 1:2], func=AF.Exp), r=[psb[2]], w=[b_dec])
                        k.op(act, lambda: A.activation(out=dec[:, 1:2], in_=ps[2][:, 3:4], func=AF.Exp), r=[psb[2]], w=[b_dec])
                        bk = proj(3)
                        k.op(dve, lambda: V.tensor_tensor(out=qkg[:, 1, :], in0=ps[bk][:, :], in1=E12[:], op=ALU.mult), r=[psb[bk], b_E], w=[b_qkg[1]])
                        ysl = tt % 2
                        for mx in range(2):
                            tb = ps[3][:, mx * 256:(mx + 1) * 256].bitcast(BF16)
                            for j in range(4):
                                k.op(pe, lambda: T.transpose(tb[:, j * 128:(j + 1) * 128], qkg[:, mx, j * 128:(j + 1) * 128], identb[:]),
                                     r=[b_qkg[mx], b_id], w=[psb[3]], inc=(j == 3))
                            k.op(act, lambda: A.copy(out=qkT[:, mx, :, :].rearrange("p a b -> p (a b)"), in_=tb), r=[psb[3]], w=[b_qkT[mx]])
                            scb = 4 + mx; outb = 6; dsb = 7
                            for hd in range(4):
                                pr, hp = hd // 2, hd % 2
                                R = slice(hp * 64, hp * 64 + 64)
                                si = mx * 2 + pr
                                mm(ps[scb][:, hd * 128:(hd + 1) * 128], qkT[R, mx, 2 + pr, :], qkT[R, mx, pr, :], r=[b_qkT[mx]], w=[psb[scb]])
                                sl = hd % 2
                                k.op(dve, lambda: V.tensor_tensor(out=scm[sl][:], in0=ps[scb][:, hd * 128:(hd + 1) * 128], in1=maskb[:], op=ALU.mult),
                                     r=[psb[scb], b_cst], w=[b_scm[sl]])
                                vv = vb[:, mx, hd * 128:(hd + 1) * 128]
                                mm(ps[outb][:, hd * 128:(hd + 1) * 128], scm[sl][:], vv, r=[b_scm[sl], b_vb[mx]], w=[psb[outb]], start=True, stop=False,
                                   inc=False)
                                mm(ps[outb][:, hd * 128:(hd + 1) * 128], qkT[R, mx, pr, :], stb[R, si, :], r=[b_qkT[mx], b_stb[si]], w=[psb[outb]],
                                   start=False, stop=True)
                                mm(ps[dsb][R, si * 128:(si + 1) * 128], qkg[:, mx, 256 + hd * 64:256 + hd * 64 + 64], vv, r=[b_qkg[mx], b_vb[mx]],
                                   w=[psb[dsb]])
                                if hp == 1:
                                    dcol = rdec[:, pr:pr + 1] if mx == 0 else dec[:, pr:pr + 1]
                                    rd = [b_cst] if mx == 0 else [b_dec]
                                    k.op(dve, lambda: V.tensor_tensor(out=stf[:, si, :], in0=ps[dsb][:, si * 128:(si + 1) * 128], in1=stf[:, si, :],
                                                                      op=ALU.add), r=[psb[dsb], b_stf[si]], w=[b_stf[si]])
                                    k.op(dve, lambda: V.tensor_scalar(out=stf[:, si, :], in0=stf[:, si, :], scalar1=dcol, scalar2=None, op0=ALU.mult),
                                         r=[b_stf[si]] + rd, w=[b_stf[si]])
                                    k.op(pool, lambda: G.tensor_copy(out=stb[:, si, :], in_=stf[:, si, :]), r=[b_stf[si]], w=[b_stb[si]])
                            for hd in range(4):
                                col = (mx * 4 + hd) * 3
                                osl = ps[outb][:, hd * 128:(hd + 1) * 128]
                                k.op(act, lambda: A.activation(out=ojk[:], in_=osl, func=AF.Square, accum_out=oss[:, col:col + 1]), r=[psb[outb]],
                                     w=[b_ojk, b_oss])
                                k.op(dve, lambda: V.tensor_scalar(out=oss[:, col + 1:col + 2], in0=oss[:, col:col + 1], scalar1=1.0 / 128, scalar2=EPS,
                                                                  op0=ALU.mult, op1=ALU.add), r=[b_oss], w=[b_oss])
                                k.op(pool, lambda: G.tensor_tensor(out=oss[:, col + 2:col + 3], in0=oss[:, col + 1:col + 2], in1=mh[:, 0:1], op=ALU.pow),
                                     r=[b_oss, b_mh], w=[b_oss])
                                k.op(dve, lambda: V.scalar_tensor_tensor(out=ybc[ysl][:, mx * 512 + hd * 128:mx * 512 + (hd + 1) * 128], in0=osl,
                                                                         scalar=oss[:, col + 2:col + 3], in1=GG[:, mx, hd * 128:(hd + 1) * 128],
                                                                         op0=ALU.mult, op1=ALU.mult), r=[psb[outb], b_oss, b_GG[mx]], w=[b_ybc[ysl]])
                        k.dma(sp, ybc_d[:, tt, :], ybc[ysl][:], r=[b_ybc[ysl]], w=[k.dbuf(('ybc', tt))])
                        if t == 1 and g + 1 < NG:
                            npipe.part1(g + 1)
                    if g + 1 < NG:
                        npipe.part2(g + 1)
                k.barrier()

        def m1c_phase(l, src, dst):
            with contextlib.ExitStack() as st:
                WG = sbt(st, "WG", [128, 8, 3072], BF16); b_wg = [Buf() for _ in range(6)]
                WBR = sbt(st, "WBR", [128, 3, 4, D], BF16); b_wbr = [Buf() for _ in range(3)]
                WO = sbt(st, "WO", [128, 8, D], BF16); b_wo = Buf()
                for pi in range(6):
                    for c in range(8):
                        k.dma(pool, WG[:, c, pi * 512:(pi + 1) * 512], W['w_in'][l, c * 128:(c + 1) * 128, 4624 + pi * 512:4624 + (pi + 1) * 512],
                              w=[b_wg[pi]])
                for b, nm in enumerate(('w_branch_a', 'w_branch_b', 'w_branch_c')):
                    for c in range(4):
                        k.dma(pool, WBR[:, b, c, :], W[nm][l, c * 128:(c + 1) * 128, :], w=[b_wbr[b]])
                for c in range(8):
                    k.dma(pool, WO[:, c, :], W['w_out'][l, c * 128:(c + 1) * 128, :], w=[b_wo])
                npipe = NormPipe(st, src, W['mix_norm'][l, :], [5])
                yin = [sbt(st, f"yin{i}", [128, 1536], BF16) for i in range(2)]; b_yin = [Buf(), Buf()]
                yT = sbt(st, "yT", [128, 12, 128], BF16); b_yT = Buf()
                sg = [sbt(st, f"sg{i}", [128, 512], F32) for i in range(2)]; b_sg = [Buf(), Buf()]
                mg = sbt(st, "mg", [128, 512], F32); b_mg = Buf()
                mt = sbt(st, "mt", [128, 512], F32); b_mt = Buf()
                mb = sbt(st, "mb", [128, D], BF16); b_mb = Buf()
                mT = sbt(st, "mT", [128, 8, 128], BF16); b_mT = Buf()
                xr = [sbt(st, f"xr{i}", [128, D], F32) for i in range(2)]; b_xr = [Buf(), Buf()]
                src_ap, src_key = src; dst_ap, dst_key = dst
                npipe.part1(0); npipe.part2(0)
                ng_ = 0
                for g in range(NG):
                    hT = npipe.hT
                    for t in range(4):
                        tt = g * 4 + t
                        ysl = tt % 2
                        k.dma(sp, yin[ysl][:, 0:512], ya_d[:, tt, :], r=[k.dbuf(('ya', g))], w=[b_yin[ysl]])
                        k.dma(sp, yin[ysl][:, 512:1536], ybc_d[:, tt, :], r=[k.dbuf(('ybc', tt))], w=[b_yin[ysl]])
                        k.dma(sp, xr[ysl][:], src_ap[tt * 128:(tt + 1) * 128, :], r=[k.dbuf((src_key, tt))], w=[b_xr[ysl]])
                        t6 = ps[6][:].bitcast(BF16); t7 = ps[7][:].bitcast(BF16)
                        for c in range(12):
                            dstp = t6[:, c * 128:(c + 1) * 128] if c < 8 else t7[:, (c - 8) * 128:(c - 7) * 128]
                            bkk = 6 if c < 8 else 7
                            k.op(pe, lambda: T.transpose(dstp, yin[ysl][:, c * 128:(c + 1) * 128], identb[:]), r=[b_yin[ysl], b_id], w=[psb[bkk]],
                                 inc=(c == 7 or c == 11))
                        k.op(act, lambda: A.copy(out=yT[:, 0:8, :].rearrange("p a b -> p (a b)"), in_=t6), r=[psb[6]], w=[b_yT])
                        k.op(dve, lambda: V.tensor_copy(out=yT[:, 8:12, :].rearrange("p a b -> p (a b)"), in_=t7[:, 0:512]), r=[psb[7]], w=[b_yT])
                        for half in range(2):
                            for b in range(3):
                                gb = ng_ % 2; bb = 2 + ng_ % 2; ng_ += 1
                                for kk in range(8):
                                    mm(ps[gb][:, :], hT[:, kk, t * 128:(t + 1) * 128], WG[:, kk, b * 1024 + half * 512:b * 1024 + (half + 1) * 512],
                                       r=[npipe.b_hT, b_wg[b * 2 + half]], w=[psb[gb]], start=(kk == 0), stop=(kk == 7), inc=(kk == 7))
                                k.op(act, lambda: A.activation(out=sg[gb][:], in_=ps[gb][:, :], func=AF.Sigmoid), r=[psb[gb]], w=[b_sg[gb]])
                                for kk in range(4):
                                    mm(ps[bb][:, :], yT[:, b * 4 + kk, :], WBR[:, b, kk, half * 512:(half + 1) * 512], r=[b_yT, b_wbr[b]], w=[psb[bb]],
                                       start=(kk == 0), stop=(kk == 3), inc=(kk == 3))
                                if b == 0:
                                    k.op(dve, lambda: V.tensor_tensor(out=mg[:], in0=sg[gb][:], in1=ps[bb][:, :], op=ALU.mult), r=[b_sg[gb], psb[bb]],
                                         w=[b_mg])
                                else:
                                    k.op(dve, lambda: V.tensor_tensor(out=mt[:], in0=sg[gb][:], in1=ps[bb][:, :], op=ALU.mult), r=[b_sg[gb], psb[bb]],
                                         w=[b_mt])
                                    if b == 1:
                                        k.op(pool, lambda: G.tensor_tensor(out=mg[:], in0=mg[:], in1=mt[:], op=ALU.add), r=[b_mg, b_mt], w=[b_mg])
                                    else:
                                        k.op(pool, lambda: G.tensor_tensor(out=mb[:, half * 512:(half + 1) * 512], in0=mg[:], in1=mt[:], op=ALU.add),
                                             r=[b_mg, b_mt], w=[b_mb])
                        for c in range(8):
                            k.op(pe, lambda: T.transpose(t6[:, c * 128:(c + 1) * 128], mb[:, c * 128:(c + 1) * 128], identb[:]), r=[b_mb, b_id],
                                 w=[psb[6]], inc=(c == 7))
                        k.op(act, lambda: A.copy(out=mT[:].rearrange("p a b -> p (a b)"), in_=t6), r=[psb[6]], w=[b_mT])
                        for half in range(2):
                            for kk in range(8):
                                mm(ps[4][:, :], mT[:, kk, :], WO[:, kk, half * 512:(half + 1) * 512], r=[b_mT, b_wo], w=[psb[4]], start=(kk == 0),
                                   stop=(kk == 7), inc=(kk == 7))
                            k.op(dve, lambda: V.tensor_tensor(out=xr[ysl][:, half * 512:(half + 1) * 512], in0=ps[4][:, :],
                                                              in1=xr[ysl][:, half * 512:(half + 1) * 512], op=ALU.add), r=[psb[4], b_xr[ysl]],
                                 w=[b_xr[ysl]])
                        k.dma(sp, dst_ap[tt * 128:(tt + 1) * 128, :], xr[ysl][:], r=[b_xr[ysl]], w=[k.dbuf((dst_key, tt))])
                        if t == 1 and g + 1 < NG:
                            npipe.part1(g + 1)
                    if g + 1 < NG:
                        npipe.part2(g + 1)
                k.barrier()

        k.barrier()
        XR = (xres, 'xres')
        for l in range(DEPTH):
            if PH is None or 'ffn1' in PH: ffn_phase(l, 1, (x_in, 'x') if l == 0 else XR, XR, False)
            if PH is None or 'm1a' in PH: m1a_phase(l, XR)
            if PH is None or 'attn' in PH: attn_phase(l)
            if PH is None or 'm1b' in PH: m1b_phase(l, XR)
            if PH is None or 'm1c' in PH: m1c_phase(l, XR, XR)
            if PH is None or 'ffn2' in PH: ffn_phase(l, 2, XR, XR, l == DEPTH - 1)
        k.barrier()
    return nc


def make_consts(S):
    NT = S // 128
    c = {}
    c['c_ident'] = np.eye(128, dtype=np.float32)
    d = 64
    inv = (10000.0 ** (-np.arange(0, d, 2, dtype=np.float32) / np.float32(d))).astype(np.float32)
    ang = (np.arange(S, dtype=np.float32)[:, None] * inv[None, :]).astype(np.float32)
    cs = np.concatenate([np.cos(ang), np.sin(ang)], axis=1).astype(np.float32)
    c['c_rope'] = np.ascontiguousarray(cs.reshape(NT, 128, 64).transpose(1, 0, 2))
    h = np.arange(4, dtype=np.float64)
    log_g = np.log1p(-np.exp2(-5.0 - h))
    pos = np.arange(128, dtype=np.float64)
    eq = np.exp(log_g[None, :] * (pos[:, None] + 1.0))
    ek = np.exp(-log_g[None, :] * (pos[:, None] + 1.0)) * 0.125
    rt = np.concatenate([np.repeat(eq, 64, axis=1), np.repeat(ek, 64, axis=1)], axis=1)
    c['c_rtab'] = rt.astype(np.float32)
    j = np.arange(128)
    up = (j[:, None] <= j[None, :])
    c['c_tri'] = (up * (-1.0 / 16.0)).astype(np.float32)
    c['c_mask'] = up.astype(np.float32)
    rd = np.zeros((128, 2), np.float64)
    for pr in range(2):
        for hp in range(2):
            rd[hp * 64:(hp + 1) * 64, pr] = np.exp(log_g[2 * pr + hp] * 128.0)
    c['c_rdec'] = rd.astype(np.float32)
    return c


_CACHE = {}


def kernel(**inputs):
    S, DEPTH, B = 8192, 4, 4
    if 'nc' not in _CACHE:
        _CACHE['nc'] = build(S, DEPTH)
    nc = _CACHE['nc']
    consts = make_consts(S)
    x = np.ascontiguousarray(np.asarray(inputs['x'], dtype=np.float32))
    shared = {n: np.ascontiguousarray(np.asarray(inputs[n], dtype=np.float32)) for n, _ in WSPEC}
    shared['final_norm'] = np.ascontiguousarray(np.asarray(inputs['final_norm'], dtype=np.float32))
    shared.update(consts)
    in_maps = []
    for c in range(B):
        m = dict(shared); m['x'] = x[c]
        in_maps.append(m)
    res = run_bass_kernel_spmd(nc, in_maps, core_ids=list(range(B)))
    return np.stack([np.asarray(r['y']).reshape(S, D) for r in res.results], axis=0).astype(np.float32)
```

```python
import math, contextlib
import numpy as np
import concourse.bass as bass, concourse.mybir as mybir
from concourse.bass_utils import run_bass_kernel_spmd
from concourse.alu_op_type import AluOpType as ALU

F32, BF16 = mybir.dt.float32, mybir.dt.bfloat16
AF = mybir.ActivationFunctionType
D = 1024; DFF = 2752; NH = 4; IN_COLS = 7696; EPS = 1e-6
STRICT_SAME = True
M1B_LA = False

WSPEC = [('ffn1_norm', (D,)), ('ffn1_w1', (D, DFF)), ('ffn1_w3', (D, DFF)), ('ffn1_w2', (DFF, D)),
         ('mix_norm', (D,)), ('w_in', (D, IN_COLS)), ('lam_qk', (4, 64)), ('da_norm', (128,)),
         ('ret_norm', (128,)), ('gla_a2', (16, 256)), ('gla_a_bias', (256,)), ('gla_norm', (128,)),
         ('w_branch_a', (512, D)), ('w_branch_b', (512, D)), ('w_branch_c', (512, D)), ('w_out', (D, D)),
         ('ffn2_norm', (D,)), ('ffn2_w1', (D, DFF)), ('ffn2_w3', (D, DFF)), ('ffn2_w2', (DFF, D))]


class Lane:
    def __init__(s, nc, es, name):
        s.sem = es.enter_context(nc.semaphore(name)); s.cnt = 0; s.name = name


class Eng(Lane):
    def __init__(s, nc, es, name, h, nlanes=0):
        super().__init__(nc, es, name)
        s.h = h; s.seen = {}
        s.lanes = [Lane(nc, es, f"{name}_l{i}") for i in range(nlanes)]; s.li = 0


class Buf:
    __slots__ = ('w', 'r')

    def __init__(s):
        s.w = None; s.r = {}


class K:
    def __init__(s, nc, es):
        s.nc = nc
        s.pe = Eng(nc, es, "pe", nc.tensor)
        s.act = Eng(nc, es, "act", nc.scalar)
        s.dve = Eng(nc, es, "dve", nc.vector)
        s.pool = Eng(nc, es, "pool", nc.gpsimd, nlanes=6)
        s.sp = Eng(nc, es, "sp", nc.sync, nlanes=10)
        s.engs = [s.pe, s.act, s.dve, s.pool, s.sp]
        s.dram = {}

    def dbuf(s, key):
        b = s.dram.get(key)
        if b is None:
            b = s.dram[key] = Buf()
        return b

    def _waits(s, eng, r, w, extra=()):
        need = {}

        def add(t):
            if t is None: return
            l, c = t
            if need.get(l, 0) < c: need[l] = c
        for b in r: add(b.w)
        for b in w:
            add(b.w)
            for l, c in b.r.items(): add((l, c))
        for t in extra: add(t)
        for l, c in need.items():
            if l is eng and (not STRICT_SAME or c > eng.cnt): continue
            if eng.seen.get(l, 0) >= c: continue
            eng.h.wait_ge(l.sem, c); eng.seen[l] = c

    def op(s, eng, fn, r=(), w=(), inc=True):
        s._waits(eng, r, w)
        inst = fn()
        tgt = eng.cnt + 1
        if inc:
            inst.then_inc(eng.sem, 1); eng.cnt = tgt
        for b in r: b.r[eng] = tgt
        for b in w:
            b.w = (eng, tgt); b.r = {}
        return inst

    def dma(s, eng, out, in_, r=(), w=(), **kw):
        lane = eng.lanes[eng.li]; eng.li = (eng.li + 1) % len(eng.lanes)
        s._waits(eng, r, w, extra=[(lane, lane.cnt)] if lane.cnt else ())
        eng.h.dma_start(out=out, in_=in_, **kw).then_inc(lane.sem, 16)
        lane.cnt += 16
        for b in r: b.r[lane] = lane.cnt
        for b in w:
            b.w = (lane, lane.cnt); b.r = {}

    def barrier(s):
        alll = []
        for e in s.engs:
            alll.append(e); alll.extend(e.lanes)
        for e in s.engs:
            for l in alll:
                if l is e or l.cnt == 0: continue
                if e.seen.get(l, 0) >= l.cnt: continue
                e.h.wait_ge(l.sem, l.cnt); e.seen[l] = l.cnt


def lam_init_of(l):
    return 0.8 - 0.6 * math.exp(-0.3 * l)


def build(S=8192, DEPTH=4, PH=None):
    NT = S // 128; NG = S // 512
    nc = bass.Bass("TRN2", target_bir_lowering=False)

    def dr(n, sh, dt=F32, kind="ExternalInput"):
        return nc.dram_tensor(n, list(sh), dt, kind=kind).ap()
    x_in = dr("x", [S, D])
    W = {n: dr(n, (DEPTH,) + sh) for n, sh in WSPEC}
    fin_g = dr("final_norm", [D])
    c_ident = dr("c_ident", [128, 128])
    c_rope = dr("c_rope", [128, NT, 64])
    c_rtab = dr("c_rtab", [128, 512])
    c_tri = dr("c_tri", [128, 128])
    c_mask = dr("c_mask", [128, 128])
    c_rdec = dr("c_rdec", [128, 2])
    y_out = dr("y", [S, D], kind="ExternalOutput")
    xres = dr("xres", [S, D], kind="Internal")
    qT_d = dr("qT_d", [4, 128, S], BF16, kind="Internal")
    kT_d = dr("kT_d", [4, 128, S], BF16, kind="Internal")
    v_d = dr("v_d", [128, NT, 4, 129], BF16, kind="Internal")
    ya_d = dr("ya_d", [128, NT, 512], BF16, kind="Internal")
    ybc_d = dr("ybc_d", [128, NT, 1024], BF16, kind="Internal")

    es = contextlib.ExitStack()
    with es:
        k = K(nc, es)
        pe, act, dve, pool, sp = k.pe, k.act, k.dve, k.pool, k.sp
        T, V, A, G = nc.tensor, nc.vector, nc.scalar, nc.gpsimd

        uid = [0]

        def sbt(stack, n, sh, dt):
            uid[0] += 1
            return stack.enter_context(nc.sbuf_tensor(f"{n}_{uid[0]}", list(sh), dt))
        psall = es.enter_context(nc.psum_tensor("psall", [128, 4096], F32))
        ps = [psall[:, i * 512:(i + 1) * 512] for i in range(8)]
        psb = [Buf() for _ in range(8)]
        identb = sbt(es, "identb", [128, 128], BF16); b_id = Buf()
        mh = sbt(es, "mh", [128, 1], F32); b_mh = Buf()
        k.dma(pool, identb[:], c_ident[:], w=[b_id])
        mh4 = sbt(es, "mh4", [128, 4], F32)
        k.op(pool, lambda: G.memset(mh[:], -0.5), w=[b_mh])
        k.op(pool, lambda: G.memset(mh4[:], -0.5), w=[b_mh])

        def mm(out, lhsT, rhs, r, w, start=True, stop=True, inc=True, **kw):
            return k.op(pe, lambda: T.matmul(out, lhsT, rhs, start=start, stop=stop, **kw), r=r, w=w, inc=inc)

        class NormPipe:
            def __init__(s, st, src, gain_ap, pst_banks):
                s.src = src
                s.xn = [sbt(st, f"xn{i}", [128, D], F32) for i in range(2)]; s.b_xn = [Buf(), Buf()]
                s.hb = [sbt(st, f"hb{i}", [128, D], BF16) for i in range(4)]; s.b_hb = [Buf() for _ in range(4)]
                s.hT = sbt(st, "hT", [128, 8, 512], BF16); s.b_hT = Buf()
                s.gB = sbt(st, "gB", [128, D], F32); s.b_gB = Buf()
                s.junk = sbt(st, "junk", [128, D], BF16); s.b_junk = Buf()
                s.ss = sbt(st, "nss", [128, 16], F32); s.b_ss = [Buf() for _ in range(4)]
                s.pst = pst_banks; s.n = 0
                k.dma(sp, s.gB[:], gain_ap.partition_broadcast(128), w=[s.b_gB])

            def part1(s, g):
                src_ap, src_key = s.src
                for t in range(4):
                    tt = g * 4 + t
                    xn = s.xn[t % 2]; bx = s.b_xn[t % 2]; ss = s.ss; bs = s.b_ss[t]
                    k.dma(sp, xn[:], src_ap[tt * 128:(tt + 1) * 128, :], r=[k.dbuf((src_key, tt))], w=[bx])
                    k.op(act, lambda: A.activation(out=s.junk[:], in_=xn[:], func=AF.Square, accum_out=ss[:, 4 * t:4 * t + 1]),
                         r=[bx], w=[s.b_junk, bs])
                    k.op(dve, lambda: V.tensor_scalar(out=ss[:, 4 * t + 1:4 * t + 2], in0=ss[:, 4 * t:4 * t + 1], scalar1=1.0 / D,
                                                      scalar2=EPS, op0=ALU.mult, op1=ALU.add), r=[bs], w=[bs])
                    k.op(pool, lambda: G.tensor_tensor(out=ss[:, 4 * t + 2:4 * t + 3], in0=ss[:, 4 * t + 1:4 * t + 2], in1=mh[:, 0:1],
                                                       op=ALU.pow), r=[bs, b_mh], w=[bs])
                    k.op(dve, lambda: V.scalar_tensor_tensor(out=s.hb[t][:], in0=xn[:], scalar=ss[:, 4 * t + 2:4 * t + 3], in1=s.gB[:],
                                                             op0=ALU.mult, op1=ALU.mult), r=[bx, bs, s.b_gB], w=[s.b_hb[t]])

            def part2(s, g):
                for t in range(4):
                    bank = s.pst[s.n % len(s.pst)]; s.n += 1
                    pst = ps[bank][:].bitcast(BF16)
                    for c in range(8):
                        k.op(pe, lambda: T.transpose(pst[:, c * 128:(c + 1) * 128], s.hb[t][:, c * 128:(c + 1) * 128], identb[:]),
                             r=[s.b_hb[t], b_id], w=[psb[bank]], inc=(c == 7))
                    src = pst.rearrange("p (c t) -> p c t", c=8)
                    if t % 2 == 0:
                        k.op(dve, lambda: V.tensor_copy(out=s.hT[:, :, t * 128:(t + 1) * 128], in_=src), r=[psb[bank]], w=[s.b_hT])
                    else:
                        k.op(act, lambda: A.copy(out=s.hT[:, :, t * 128:(t + 1) * 128], in_=src), r=[psb[bank]], w=[s.b_hT])

        def wload(dst, src_rows_fn, nk, ncols, piece, bufs):
            npieces = (ncols + piece - 1) // piece
            for pi in range(npieces):
                c0 = pi * piece; c1 = min(ncols, c0 + piece)
                for c in range(nk):
                    src = src_rows_fn(c)
                    rows = src.shape[0]
                    k.dma(pool, dst[0:rows, c, c0:c1], src[:, c0:c1], w=[bufs[pi]])

        def ffn_phase(l, which, src, dst, final):
            pre = 'ffn1' if which == 1 else 'ffn2'
            with contextlib.ExitStack() as st:
                WS = sbt(st, "WS", [128, 8 * DFF * 2 + 22 * D], BF16)
                W1 = WS[:, 0:8 * DFF].rearrange("p (c f) -> p c f", c=8)
                W3 = WS[:, 8 * DFF:16 * DFF].rearrange("p (c f) -> p c f", c=8)
                W2 = WS[:, 16 * DFF:16 * DFF + 22 * D].rearrange("p (c f) -> p c f", c=22)
                PIECE = 1408
                b_w1 = [Buf(), Buf()]; b_w3 = [Buf(), Buf()]; b_w2 = [Buf()]
                w1d, w3d, w2d = W[pre + '_w1'], W[pre + '_w3'], W[pre + '_w2']
                for pi in range(2):
                    c0 = pi * PIECE; c1 = min(DFF, c0 + PIECE)
                    for (wd, Wt, bb) in ((w1d, W1, b_w1), (w3d, W3, b_w3)):
                        for c in range(8):
                            k.dma(pool, Wt[:, c, c0:c1], wd[l, c * 128:(c + 1) * 128, c0:c1], w=[bb[pi]])
                for c in range(22):
                    rows = 128 if c < 21 else 64
                    k.dma(pool, W2[0:rows, c, :], w2d[l, c * 128:c * 128 + rows, :], w=[b_w2[0]])
                npipe = NormPipe(st, src, W[pre + '_norm'][l, :], [6, 7])
                u = sbt(st, "u", [128, 22, 512], BF16); b_u = [Buf() for _ in range(22)]
                sa = [sbt(st, f"sa{i}", [128, 512], F32) for i in range(2)]; b_sa = [Buf(), Buf()]
                xr = [sbt(st, f"xr{i}", [128, D], F32) for i in range(2)]; b_xr = [Buf(), Buf()]
                if final:
                    gF = sbt(st, "gF", [128, D], F32); b_gF = Buf()
                    k.dma(sp, gF[:], fin_g.partition_broadcast(128), w=[b_gF])
                    fss = sbt(st, "fss", [128, 8], F32); b_fss = [Buf(), Buf()]
                src_ap, src_key = src
                dst_ap, dst_key = dst
                npipe.part1(0); npipe.part2(0)
                nxr = 0
                for g in range(NG):
                    for f in range(22):
                        fs = 128 if f < 21 else 64
                        pa = f % 2; pb = 2 + f % 2
                        pi = 0 if f * 128 < PIECE else 1
                        for kk in range(8):
                            mm(ps[pa][0:fs, :], W1[:, kk, f * 128:f * 128 + fs], npipe.hT[:, kk, :], r=[npipe.b_hT, b_w1[pi]],
                               w=[psb[pa]], start=(kk == 0), stop=(kk == 7), inc=(kk == 7))
                        for kk in range(8):
                            mm(ps[pb][0:fs, :], W3[:, kk, f * 128:f * 128 + fs], npipe.hT[:, kk, :], r=[npipe.b_hT, b_w3[pi]],
                               w=[psb[pb]], start=(kk == 0), stop=(kk == 7), inc=(kk == 7))
                        k.op(act, lambda: A.activation(out=sa[f % 2][0:fs, :], in_=ps[pa][0:fs, :], func=AF.Silu),
                             r=[psb[pa]], w=[b_sa[f % 2]])
                        k.op(dve, lambda: V.tensor_tensor(out=u[0:fs, f, :], in0=sa[f % 2][0:fs, :], in1=ps[pb][0:fs, :], op=ALU.mult),
                             r=[b_sa[f % 2], psb[pb]], w=[b_u[f]])
                        if f == 8 and g + 1 < NG:
                            npipe.part1(g + 1)
                    if g + 1 < NG:
                        npipe.part2(g + 1)
                    for t in range(4):
                        tt = g * 4 + t
                        xs = nxr % 2; nxr += 1
                        k.dma(sp, xr[xs][:], src_ap[tt * 128:(tt + 1) * 128, :], r=[k.dbuf((src_key, tt))], w=[b_xr[xs]])
                        for half in range(2):
                            py = 4 + half
                            for f in range(22):
                                fs = 128 if f < 21 else 64
                                mm(ps[py][:, :], u[0:fs, f, t * 128:(t + 1) * 128], W2[0:fs, f, half * 512:(half + 1) * 512],
                                   r=[b_u[f], b_w2[0]], w=[psb[py]], start=(f == 0), stop=(f == 21), inc=(f == 21))
                            k.op(dve, lambda: V.scalar_tensor_tensor(out=xr[xs][:, half * 512:(half + 1) * 512], in0=ps[py][:, :], scalar=0.5,
                                                                     in1=xr[xs][:, half * 512:(half + 1) * 512], op0=ALU.mult, op1=ALU.add),
                                 r=[psb[py], b_xr[xs]], w=[b_xr[xs]])
                        if not final:
                            k.dma(sp, dst_ap[tt * 128:(tt + 1) * 128, :], xr[xs][:], r=[b_xr[xs]], w=[k.dbuf((dst_key, tt))])
                        else:
                            k.op(act, lambda: A.activation(out=npipe.junk[:], in_=xr[xs][:], func=AF.Square, accum_out=fss[:, 4 * xs:4 * xs + 1]),
                                 r=[b_xr[xs]], w=[npipe.b_junk, b_fss[xs]])
                            k.op(dve, lambda: V.tensor_scalar(out=fss[:, 4 * xs + 1:4 * xs + 2], in0=fss[:, 4 * xs:4 * xs + 1], scalar1=1.0 / D,
                                                              scalar2=EPS, op0=ALU.mult, op1=ALU.add), r=[b_fss[xs]], w=[b_fss[xs]])
                            k.op(pool, lambda: G.tensor_tensor(out=fss[:, 4 * xs + 2:4 * xs + 3], in0=fss[:, 4 * xs + 1:4 * xs + 2],
                                                               in1=mh[:, 0:1], op=ALU.pow), r=[b_fss[xs], b_mh], w=[b_fss[xs]])
                            k.op(dve, lambda: V.scalar_tensor_tensor(out=xr[xs][:], in0=xr[xs][:], scalar=fss[:, 4 * xs + 2:4 * xs + 3],
                                                                     in1=gF[:], op0=ALU.mult, op1=ALU.mult),
                                 r=[b_xr[xs], b_fss[xs], b_gF], w=[b_xr[xs]])
                            k.dma(sp, y_out[tt * 128:(tt + 1) * 128, :], xr[xs][:], r=[b_xr[xs]], w=[k.dbuf(('y', tt))])
                k.barrier()

        def m1a_phase(l, src):
            with contextlib.ExitStack() as st:
                WA = sbt(st, "WA", [128, 8, 1536], BF16); b_wa = [Buf(), Buf(), Buf()]
                wload(WA, lambda c: W['w_in'][l, c * 128:(c + 1) * 128, 0:1536], 8, 1536, 512, b_wa)
                npipe = NormPipe(st, src, W['mix_norm'][l, :], [6, 7])
                qk = [sbt(st, f"qk{i}", [128, 512], BF16) for i in range(2)]; b_qk = [Buf(), Buf()]
                vs = [sbt(st, f"vs{i}", [128, 4, 4, 129], BF16) for i in range(2)]; b_vs = [Buf(), Buf()]
                for i in range(2):
                    k.op(pool, lambda: G.memset(vs[i][:], 1.0), w=[b_vs[i]])
                npipe.part1(0); npipe.part2(0)
                for g in range(NG):
                    for c in range(8):
                        pb_ = c % 2
                        for kk in range(8):
                            mm(ps[pb_][:, :], WA[:, kk, c * 128:(c + 1) * 128], npipe.hT[:, kk, :], r=[npipe.b_hT, b_wa[c // 4]],
                               w=[psb[pb_]], start=(kk == 0), stop=(kk == 7), inc=(kk == 7))
                        if c % 2 == 0:
                            k.op(act, lambda: A.copy(out=qk[pb_][:], in_=ps[pb_][:, :]), r=[psb[pb_]], w=[b_qk[pb_]])
                        else:
                            k.op(dve, lambda: V.tensor_copy(out=qk[pb_][:], in_=ps[pb_][:, :]), r=[psb[pb_]], w=[b_qk[pb_]])
                        dd, key = (qT_d, 'qT') if c < 4 else (kT_d, 'kT')
                        k.dma(sp, dd[c % 4, :, g * 512:(g + 1) * 512], qk[pb_][:], r=[b_qk[pb_]], w=[k.dbuf((key, c % 4, g))])
                        if c == 3 and g + 1 < NG:
                            npipe.part1(g + 1)
                    vsl = g % 2
                    for t in range(4):
                        pv = 2 + t % 2
                        for kk in range(8):
                            mm(ps[pv][:, :], npipe.hT[:, kk, t * 128:(t + 1) * 128], WA[:, kk, 1024:1536], r=[npipe.b_hT, b_wa[2]],
                               w=[psb[pv]], start=(kk == 0), stop=(kk == 7), inc=(kk == 7))
                        src_v = ps[pv][:, :].rearrange("p (h e) -> p h e", h=4)
                        if t % 2 == 0:
                            k.op(act, lambda: A.copy(out=vs[vsl][:, t, :, 0:128], in_=src_v), r=[psb[pv]], w=[b_vs[vsl]])
                        else:
                            k.op(dve, lambda: V.tensor_copy(out=vs[vsl][:, t, :, 0:128], in_=src_v), r=[psb[pv]], w=[b_vs[vsl]])
                    k.dma(sp, v_d[:, g * 4:(g + 1) * 4, :, :], vs[vsl][:], r=[b_vs[vsl]], w=[k.dbuf(('v', g))])
                    if g + 1 < NG:
                        npipe.part2(g + 1)
                k.barrier()

        def attn_phase(l):
            li = lam_init_of(l)
            with contextlib.ExitStack() as st:
                kT = sbt(st, "kT", [128, 4, S], BF16); b_kT = [Buf() for _ in range(4)]
                va = sbt(st, "va", [128, NT, 4, 129], BF16); b_va = Buf()
                qg = [sbt(st, f"qg{i}", [128, 4, 512], BF16) for i in range(2)]; b_qg = [Buf(), Buf()]
                Pt = [sbt(st, f"Pt{i}", [128, 512], BF16) for i in range(4)]; b_P = [Buf() for _ in range(4)]
                yag = [sbt(st, f"yag{i}", [128, 4, 512], BF16) for i in range(2)]; b_yag = [Buf(), Buf()]
                lq = sbt(st, "lq", [128, 256], F32); b_lq = Buf()
                lt = sbt(st, "lt", [128, 8], F32); b_lt = Buf()
                ljunk = sbt(st, "ljunk", [128, 64], F32); b_lj = Buf()
                gda = sbt(st, "gda", [128, 128], F32); b_gda = Buf()
                sm = sbt(st, "sm", [128, 12], F32); b_sm = Buf()
                sm2 = sbt(st, "sm2", [128, 12], F32); b_sm2 = Buf()
                tmp = sbt(st, "atmp", [128, 128], F32); b_tmp = Buf()
                o = sbt(st, "ao", [128, 128], F32); b_o = Buf()
                oj = sbt(st, "aoj", [128, 128], BF16); b_oj = Buf()
                for hd in range(4):
                    k.dma(sp, kT[:, hd, :], kT_d[hd, :, :], r=[k.dbuf(('kT', hd, g)) for g in range(NG)], w=[b_kT[hd]])
                k.dma(sp, va[:], v_d[:], r=[k.dbuf(('v', g)) for g in range(NG)], w=[b_va])
                k.dma(sp, lq[:], W['lam_qk'][l].rearrange("a b -> (a b)").partition_broadcast(128), w=[b_lq])
                k.dma(sp, gda[:], W['da_norm'][l, :].partition_broadcast(128), w=[b_gda])
                for i_ in range(2):
                    k.op(dve, lambda: V.tensor_tensor(out=ljunk[:], in0=lq[:, i_ * 128:i_ * 128 + 64], in1=lq[:, i_ * 128 + 64:i_ * 128 + 128],
                                                      op=ALU.mult), r=[b_lq], w=[b_lj])
                    k.op(act, lambda: A.activation(out=ljunk[:], in_=ljunk[:], func=AF.Copy, accum_out=lt[:, i_:i_ + 1]), r=[b_lj], w=[b_lj, b_lt])
                k.op(act, lambda: A.activation(out=lt[:, 2:4], in_=lt[:, 0:2], func=AF.Exp), r=[b_lt], w=[b_lt])
                k.op(dve, lambda: V.tensor_tensor(out=lt[:, 4:5], in0=lt[:, 3:4], in1=lt[:, 2:3], op=ALU.subtract), r=[b_lt], w=[b_lt])
                k.op(dve, lambda: V.tensor_scalar(out=lt[:, 5:6], in0=lt[:, 4:5], scalar1=-li, scalar2=None, op0=ALU.add), r=[b_lt], w=[b_lt])
                k.op(dve, lambda: V.tensor_scalar(out=gda[:], in0=gda[:], scalar1=(1.0 - li), scalar2=None, op0=ALU.mult), r=[b_gda], w=[b_gda])
                accS = [sbt(st, f"accS{i}", [128, 2, 2, 258], F32) for i in range(2)]; b_accS = [Buf(), Buf()]
                o4 = sbt(st, "o4", [128, 4, 128], F32); b_o4 = Buf()
                sq = sbt(st, "sq", [128, 128], F32); b_sq = Buf()
                P3 = [sbt(st, f"P3_{i}", [128, 2, 512], BF16) for i in range(3)]; b_P3 = [Buf() for _ in range(3)]
                blocks = [(Gq, hd, kb) for Gq in range(NG) for hd in range(4) for kb in range(4 * (Gq + 1))]
                nfin = [0]

                def s1(i):
                    Gq, hd, kb = blocks[i]
                    qs = Gq % 2
                    if hd == 0 and kb == 0:
                        k.dma(sp, qg[qs][:], qT_d[:, :, Gq * 512:(Gq + 1) * 512].rearrange("h p s -> p h s"),
                              r=[k.dbuf(('qT', h_, Gq)) for h_ in range(4)], w=[b_qg[qs]])
                    dg = kb - 4 * Gq; t0 = max(dg, 0); q0 = t0 * 128
                    b0 = 4 + 2 * (i % 2)
                    for m in range(2):
                        mm(ps[b0 + m][:, q0:512], kT[m * 64:(m + 1) * 64, hd, kb * 128:(kb + 1) * 128],
                           qg[qs][m * 64:(m + 1) * 64, hd, q0:512], r=[b_kT[hd], b_qg[qs]], w=[psb[b0 + m]])
                    pslot = i % 3
                    src = psall[:, b0 * 512:(b0 + 2) * 512].rearrange("p (m q) -> p m q", m=2)[:, :, q0:512]
                    k.op(act, lambda: A.activation(out=P3[pslot][:, :, q0:512], in_=src, func=AF.Exp, scale=0.125),
                         r=[psb[b0], psb[b0 + 1]], w=[b_P3[pslot]])
                    if dg >= 0:
                        k.op(pool, lambda: G.memset(P3[pslot][64:128, :, q0:q0 + 64], 0.0), w=[b_P3[pslot]])

                def s2(i):
                    Gq, hd, kb = blocks[i]
                    dg = kb - 4 * Gq; t0 = max(dg, 0)
                    pslot = i % 3
                    for m in range(2):
                        for t in range(t0, 4):
                            ab = m * 2 + t // 2
                            acc = ps[ab][:, (t % 2) * 129:(t % 2) * 129 + 129]
                            mm(acc, P3[pslot][:, m, t * 128:(t + 1) * 128], va[:, kb, hd, :], r=[b_P3[pslot], b_va], w=[psb[ab]],
                               start=(kb == 0 and t % 2 == 0), stop=(kb == 4 * Gq + t), inc=(m == 1 and t == 3), skip_group_check=True)
                    if kb == 4 * Gq + 3:
                        finalize(Gq, hd)

                def finalize(Gq, hd):
                    ys = Gq % 2
                    asl = nfin[0] % 2; nfin[0] += 1
                    aS = accS[asl]; bA = b_accS[asl]
                    for m in range(2):
                        src = psall[:, (2 * m) * 512:(2 * m + 2) * 512].rearrange("p (b q) -> p b q", b=2)[:, :, 0:258]
                        k.op(dve, lambda: V.tensor_copy(out=aS[:, m, :, :], in_=src), r=[psb[2 * m], psb[2 * m + 1]], w=[bA])
                    a5 = aS[:].rearrange("p m b (t e) -> p m b t e", t=2)
                    lcol = a5[:, :, :, :, 128:129].rearrange("p m b t e -> p (m b t e)")
                    k.op(dve, lambda: V.reciprocal(out=sm[:, 0:8], in_=lcol), r=[bA], w=[b_sm])
                    k.op(dve, lambda: V.tensor_scalar(out=sm[:, 8:12], in0=sm[:, 4:8], scalar1=lt[:, 5:6], scalar2=None, op0=ALU.mult),
                         r=[b_sm, b_lt], w=[b_sm])
                    for t in range(4):
                        a0 = a5[:, 0, t // 2, t % 2, 0:128]; a1 = a5[:, 1, t // 2, t % 2, 0:128]
                        k.op(dve, lambda: V.tensor_scalar(out=tmp[:], in0=a1, scalar1=sm[:, 8 + t:9 + t], scalar2=None, op0=ALU.mult),
                             r=[bA, b_sm], w=[b_tmp])
                        k.op(dve, lambda: V.scalar_tensor_tensor(out=o4[:, t, :], in0=a0, scalar=sm[:, t:t + 1], in1=tmp[:], op0=ALU.mult,
                                                                 op1=ALU.add), r=[bA, b_sm, b_tmp], w=[b_o4])
                        k.op(dve, lambda: V.scalar_tensor_tensor(out=sq[:], in0=o4[:, t, :], scalar=1.0, in1=o4[:, t, :], op0=ALU.mult,
                                                                 op1=ALU.mult, accum_out=sm2[:, t:t + 1]), r=[b_o4], w=[b_sq, b_sm2])
                    k.op(dve, lambda: V.tensor_scalar(out=sm2[:, 4:8], in0=sm2[:, 0:4], scalar1=1.0 / 128, scalar2=EPS, op0=ALU.mult,
                                                      op1=ALU.add), r=[b_sm2], w=[b_sm2])
                    k.op(pool, lambda: G.tensor_tensor(out=sm2[:, 8:12], in0=sm2[:, 4:8], in1=mh4[:, 0:4], op=ALU.pow), r=[b_sm2, b_mh], w=[b_sm2])
                    for t in range(4):
                        k.op(dve, lambda: V.scalar_tensor_tensor(out=yag[ys][:, t, hd * 128:(hd + 1) * 128], in0=o4[:, t, :],
                                                                 scalar=sm2[:, 8 + t:9 + t], in1=gda[:], op0=ALU.mult, op1=ALU.mult),
                             r=[b_o4, b_sm2, b_gda], w=[b_yag[ys]])
                    if hd == 3:
                        k.dma(sp, ya_d[:, Gq * 4:(Gq + 1) * 4, :], yag[ys][:], r=[b_yag[ys]], w=[k.dbuf(('ya', Gq))])

                LA = 2
                for i in range(len(blocks) + LA):
                    if i < len(blocks): s1(i)
                    if i >= LA: s2(i - LA)
                k.barrier()

        def m1b_phase(l, src):
            NC_B = 3088
            with contextlib.ExitStack() as st:
                WB = sbt(st, "WB", [128, 8, NC_B], BF16); b_wb = [Buf() for _ in range(7)]
                offs = [0, 512, 1024, 1536, 2048, 2560, 3072, 3088]
                for pi in range(7):
                    for c in range(8):
                        k.dma(pool, WB[:, c, offs[pi]:offs[pi + 1]], W['w_in'][l, c * 128:(c + 1) * 128, 1536 + offs[pi]:1536 + offs[pi + 1]],
                              w=[b_wb[pi]])
                npipe = NormPipe(st, src, W['mix_norm'][l, :], [3])
                rope = sbt(st, "rope", [128, NT, 64], F32); b_rope = Buf()
                rtab = sbt(st, "rtab", [128, 512], F32); b_rtab = Buf()
                tri = sbt(st, "tri", [128, 128], F32); maskb = sbt(st, "maskb", [128, 128], F32); b_cst = Buf()
                rdec = sbt(st, "rdec", [128, 2], F32)
                a2a = sbt(st, "a2a", [32, 256], F32); b_a2 = Buf()
                gn = sbt(st, "gn", [128, 2, 128], F32); b_gn = Buf()
                k.dma(sp, rope[:], c_rope[:], w=[b_rope])
                k.dma(sp, rtab[:], c_rtab[:], w=[b_rtab])
                k.dma(sp, tri[:], c_tri[:], w=[b_cst]); k.dma(sp, maskb[:], c_mask[:], w=[b_cst]); k.dma(sp, rdec[:], c_rdec[:], w=[b_cst])
                k.op(pool, lambda: G.memset(a2a[:], 0.0), w=[b_a2])
                k.dma(sp, a2a[0:16, :], W['gla_a2'][l], w=[b_a2])
                k.dma(sp, a2a[16:17, :], W['gla_a_bias'][l:l + 1, :], w=[b_a2])
                k.dma(sp, gn[:, 0, :], W['ret_norm'][l, :].partition_broadcast(128), w=[b_gn])
                k.dma(sp, gn[:, 1, :], W['gla_norm'][l, :].partition_broadcast(128), w=[b_gn])
                caT = sbt(st, "caT", [32, 512], F32); b_caT = Buf()
                vb2 = [sbt(st, f"vb{i}", [128, 2, 512], BF16) for i in range(2)]; b_vb2 = [[Buf(), Buf()] for _ in range(2)]
                GG2 = [sbt(st, f"GG{i}", [128, 2, 512], F32) for i in range(2)]; b_GG2 = [[Buf(), Buf()] for _ in range(2)]
                rt = sbt(st, "rt", [128, 6, 256], F32); b_rt = Buf()
                qkr = sbt(st, "qkr", [128, 512], F32); b_qkr = Buf()
                qkg2 = [sbt(st, f"qkg{i}", [128, 2, 512], BF16) for i in range(2)]; b_qkg2 = [[Buf(), Buf()] for _ in range(2)]
                spz = sbt(st, "spz", [128, 256], F32); b_spz = Buf()
                E12 = sbt(st, "E12", [128, 512], F32); b_E = Buf()
                dec2 = [sbt(st, f"dec{i}", [128, 2], F32) for i in range(2)]; b_dec2 = [Buf(), Buf()]
                qkT = sbt(st, "qkT", [128, 2, 4, 128], BF16); b_qkT = [Buf(), Buf()]
                scm = [sbt(st, f"scm{i}", [128, 128], BF16) for i in range(2)]; b_scm = [Buf(), Buf()]
                stf = sbt(st, "stf", [128, 4, 128], F32); b_stf = [Buf() for _ in range(4)]
                stb = sbt(st, "stb", [128, 4, 128], BF16); b_stb = [Buf() for _ in range(4)]
                oss = sbt(st, "oss", [128, 24], F32); b_oss = Buf()
                ojk = sbt(st, "ojk", [128, 128], BF16); b_ojk = Buf()
                ybc = [sbt(st, f"ybc{i}", [128, 1024], BF16) for i in range(2)]; b_ybc = [Buf(), Buf()]
                for i in range(4):
                    k.op(pool, lambda: G.memset(stf[:, i, :], 0.0), w=[b_stf[i]])
                    k.op(pool, lambda: G.memset(stb[:, i, :], 0.0), w=[b_stb[i]])
                k.op(pool, lambda: G.memset(caT[:], 1.0), w=[b_caT])
                npj = 0
                hT = npipe.hT

                def group_pre(g):
                    for kk in range(8):
                        mm(ps[2][0:16, :], WB[:, kk, 3072:3088], hT[:, kk, :], r=[npipe.b_hT, b_wb[6]], w=[psb[2]], start=(kk == 0), stop=(kk == 7),
                           inc=(kk == 7))
                    k.op(dve, lambda: V.tensor_copy(out=caT[0:16, :], in_=ps[2][0:16, :]), r=[psb[2]], w=[b_caT])

                def P(tt):
                    g, t = tt // 4, tt % 4; s_ = tt % 2
                    vb = vb2[s_]; b_vb = b_vb2[s_]; GG = GG2[s_]; b_GG = b_GG2[s_]
                    qkg = qkg2[s_]; b_qkg = b_qkg2[s_]; dec = dec2[s_]; b_dec = b_dec2[s_]
                    lhs = lambda kk: hT[:, kk, t * 128:(t + 1) * 128]

                    def proj(pi):
                        nonlocal npj
                        bank = npj % 2; npj += 1
                        for kk in range(8):
                            mm(ps[bank][:, :], lhs(kk), WB[:, kk, offs[pi]:offs[pi + 1]], r=[npipe.b_hT, b_wb[pi]], w=[psb[bank]],
                               start=(kk == 0), stop=(kk == 7), inc=(kk == 7))
                        return bank
                    for mx, pi in ((0, 1), (1, 4)):
                        bk = proj(pi)
                        k.op(act, lambda: A.copy(out=vb[:, mx, :], in_=ps[bk][:, :]), r=[psb[bk]], w=[b_vb[mx]])
                    for mx, pi in ((0, 2), (1, 5)):
                        bk = proj(pi)
                        k.op(act, lambda: A.activation(out=GG[:, mx, :], in_=ps[bk][:, :], func=AF.Silu), r=[psb[bk]], w=[b_GG[mx]])
                        k.op(pool, lambda: G.tensor_tensor(out=GG[:, mx, :].rearrange("p (h e) -> p h e", h=4),
                                                           in0=GG[:, mx, :].rearrange("p (h e) -> p h e", h=4),
                                                           in1=gn[:, mx, :].unsqueeze(1).broadcast_to([128, 4, 128]), op=ALU.mult),
                             r=[b_GG[mx], b_gn], w=[b_GG[mx]])
                    bk = proj(0)
                    p3 = ps[bk][:, :].rearrange("p (a two d) -> p a two d", a=8, two=2)
                    cosb = rope[:, tt, 0:32].unsqueeze(1).broadcast_to([128, 8, 32])
                    sinb = rope[:, tt, 32:64].unsqueeze(1).broadcast_to([128, 8, 32])
                    r3 = lambda i: rt[:, i, :].rearrange("p (a d) -> p a d", a=8)
                    q3 = qkr[:].rearrange("p (a two d) -> p a two d", a=8, two=2)
                    rr_ = [psb[bk], b_rope]
                    k.op(dve, lambda: V.tensor_tensor(out=r3(0), in0=p3[:, :, 0, :], in1=cosb, op=ALU.mult), r=rr_, w=[b_rt])
                    k.op(dve, lambda: V.tensor_tensor(out=r3(1), in0=p3[:, :, 1, :], in1=sinb, op=ALU.mult), r=rr_, w=[b_rt])
                    k.op(dve, lambda: V.tensor_tensor(out=r3(2), in0=p3[:, :, 0, :], in1=sinb, op=ALU.mult), r=rr_, w=[b_rt])
                    k.op(dve, lambda: V.tensor_tensor(out=r3(3), in0=p3[:, :, 1, :], in1=cosb, op=ALU.mult), r=rr_, w=[b_rt])
                    k.op(dve, lambda: V.tensor_tensor(out=q3[:, :, 0, :], in0=r3(0), in1=r3(1), op=ALU.subtract), r=[b_rt], w=[b_qkr])
                    k.op(dve, lambda: V.tensor_tensor(out=q3[:, :, 1, :], in0=r3(2), in1=r3(3), op=ALU.add), r=[b_rt], w=[b_qkr])
                    k.op(dve, lambda: V.tensor_tensor(out=qkg[:, 0, :], in0=qkr[:], in1=rtab[:], op=ALU.mult), r=[b_qkr, b_rtab], w=[b_qkg[0]])
                    mm(ps[2][:, 0:256], caT[0:32, t * 128:(t + 1) * 128], a2a[0:32, :], r=[b_caT, b_a2], w=[psb[2]])
                    k.op(act, lambda: A.activation(out=spz[:], in_=ps[2][:, 0:256], func=AF.Exp, scale=-1.0), r=[psb[2]], w=[b_spz])
                    k.op(act, lambda: A.activation(out=spz[:], in_=spz[:], func=AF.Ln, bias=1.0), r=[b_spz], w=[b_spz])
                    mm(ps[2][:, 256:512], tri[:], spz[:], r=[b_cst, b_spz], w=[psb[2]])
                    for pr in range(2):
                        mm(ps[2][:, 2 * pr:2 * pr + 2], spz[:, pr * 128:(pr + 1) * 128], tri[:, 126:128], r=[b_cst, b_spz], w=[psb[2]],
                           inc=(pr == 1))
                    k.op(act, lambda: A.activation(out=E12[:, 0:256], in_=ps[2][:, 256:512], func=AF.Exp, bias=math.log(0.125)),
                         r=[psb[2]], w=[b_E])
                    k.op(act, lambda: A.activation(out=E12[:, 256:512], in_=ps[2][:, 256:512], func=AF.Exp, scale=-1.0), r=[psb[2]], w=[b_E])
                    k.op(act, lambda: A.activation(out=dec[:, 0:1], in_=ps[2][:, 1:2], func=AF.Exp), r=[psb[2]], w=[b_dec])
                    k.op(act, lambda: A.activation(out=dec[:, 1:2], in_=ps[2][:, 3:4], func=AF.Exp), r=[psb[2]], w=[b_dec])
                    bk = proj(3)
                    k.op(dve, lambda: V.tensor_tensor(out=qkg[:, 1, :], in0=ps[bk][:, :], in1=E12[:], op=ALU.mult), r=[psb[bk], b_E], w=[b_qkg[1]])

                def L(tt):
                    g, t = tt // 4, tt % 4; s_ = tt % 2
                    vb = vb2[s_]; b_vb = b_vb2[s_]; GG = GG2[s_]; b_GG = b_GG2[s_]
                    qkg = qkg2[s_]; b_qkg = b_qkg2[s_]; dec = dec2[s_]; b_dec = b_dec2[s_]
                    ysl = tt % 2
                    for mx in range(2):
                        tb = ps[3][:, mx * 256:(mx + 1) * 256].bitcast(BF16)
                        for j in range(4):
                            k.op(pe, lambda: T.transpose(tb[:, j * 128:(j + 1) * 128], qkg[:, mx, j * 128:(j + 1) * 128], identb[:]),
                                 r=[b_qkg[mx], b_id], w=[psb[3]], inc=(j == 3))
                        k.op(act, lambda: A.copy(out=qkT[:, mx, :, :].rearrange("p a b -> p (a b)"), in_=tb), r=[psb[3]], w=[b_qkT[mx]])
                        scb = 4 + mx; outb = 6; dsb = 7
                        for hd in range(4):
                            pr, hp = hd // 2, hd % 2
                            R = slice(hp * 64, hp * 64 + 64)
                            si = mx * 2 + pr
                            mm(ps[scb][:, hd * 128:(hd + 1) * 128], qkT[R, mx, 2 + pr, :], qkT[R, mx, pr, :], r=[b_qkT[mx]], w=[psb[scb]])
                            sl = hd % 2
                            k.op(dve, lambda: V.tensor_tensor(out=scm[sl][:], in0=ps[scb][:, hd * 128:(hd + 1) * 128], in1=maskb[:], op=ALU.mult),
                                 r=[psb[scb], b_cst], w=[b_scm[sl]])
                            vv = vb[:, mx, hd * 128:(hd + 1) * 128]
                            mm(ps[outb][:, hd * 128:(hd + 1) * 128], scm[sl][:], vv, r=[b_scm[sl], b_vb[mx]], w=[psb[outb]], start=True, stop=False,
                               inc=False)
                            mm(ps[outb][:, hd * 128:(hd + 1) * 128], qkT[R, mx, pr, :], stb[R, si, :], r=[b_qkT[mx], b_stb[si]], w=[psb[outb]],
                               start=False, stop=True)
                            mm(ps[dsb][R, si * 128:(si + 1) * 128], qkg[:, mx, 256 + hd * 64:256 + hd * 64 + 64], vv, r=[b_qkg[mx], b_vb[mx]],
                               w=[psb[dsb]])
                            if hp == 1:
                                dcol = rdec[:, pr:pr + 1] if mx == 0 else dec[:, pr:pr + 1]
                                rd = [b_cst] if mx == 0 else [b_dec]
                                k.op(dve, lambda: V.tensor_tensor(out=stf[:, si, :], in0=ps[dsb][:, si * 128:(si + 1) * 128], in1=stf[:, si, :],
                                                                  op=ALU.add), r=[psb[dsb], b_stf[si]], w=[b_stf[si]])
                                k.op(dve, lambda: V.tensor_scalar(out=stf[:, si, :], in0=stf[:, si, :], scalar1=dcol, scalar2=None, op0=ALU.mult),
                                     r=[b_stf[si]] + rd, w=[b_stf[si]])
                                k.op(pool, lambda: G.tensor_copy(out=stb[:, si, :], in_=stf[:, si, :]), r=[b_stf[si]], w=[b_stb[si]])
                        for hd in range(4):
                            col = (mx * 4 + hd) * 3
                            osl = ps[outb][:, hd * 128:(hd + 1) * 128]
                            k.op(act, lambda: A.activation(out=ojk[:], in_=osl, func=AF.Square, accum_out=oss[:, col:col + 1]), r=[psb[outb]],
                                 w=[b_ojk, b_oss])
                            k.op(dve, lambda: V.tensor_scalar(out=oss[:, col + 1:col + 2], in0=oss[:, col:col + 1], scalar1=1.0 / 128, scalar2=EPS,
                                                              op0=ALU.mult, op1=ALU.add), r=[b_oss], w=[b_oss])
                            k.op(pool, lambda: G.tensor_tensor(out=oss[:, col + 2:col + 3], in0=oss[:, col + 1:col + 2], in1=mh[:, 0:1], op=ALU.pow),
                                 r=[b_oss, b_mh], w=[b_oss])
                            k.op(dve, lambda: V.scalar_tensor_tensor(out=ybc[ysl][:, mx * 512 + hd * 128:mx * 512 + (hd + 1) * 128], in0=osl,
                                                                     scalar=oss[:, col + 2:col + 3], in1=GG[:, mx, hd * 128:(hd + 1) * 128],
                                                                     op0=ALU.mult, op1=ALU.mult), r=[psb[outb], b_oss, b_GG[mx]], w=[b_ybc[ysl]])
                    k.dma(sp, ybc_d[:, tt, :], ybc[ysl][:], r=[b_ybc[ysl]], w=[k.dbuf(('ybc', tt))])

                npipe.part1(0); npipe.part2(0)
                group_pre(0)
                P(0)
                for tt in range(NT):
                    g, t = tt // 4, tt % 4
                    if tt + 1 < NT:
                        if t == 3:
                            npipe.part2(g + 1)
                            group_pre(g + 1)
                        P(tt + 1)
                    L(tt)
                    if t == 1 and g + 1 < NG:
                        npipe.part1(g + 1)
                k.barrier()

        def m1c_phase(l, src, dst):
            with contextlib.ExitStack() as st:
                WG = sbt(st, "WG", [128, 8, 3072], BF16); b_wg = [Buf() for _ in range(6)]
                WBR = sbt(st, "WBR", [128, 3, 4, D], BF16); b_wbr = [Buf() for _ in range(3)]
                WO = sbt(st, "WO", [128, 8, D], BF16); b_wo = Buf()
                for pi in range(6):
                    for c in range(8):
                        k.dma(pool, WG[:, c, pi * 512:(pi + 1) * 512], W['w_in'][l, c * 128:(c + 1) * 128, 4624 + pi * 512:4624 + (pi + 1) * 512],
                              w=[b_wg[pi]])
                for b, nm in enumerate(('w_branch_a', 'w_branch_b', 'w_branch_c')):
                    for c in range(4):
                        k.dma(pool, WBR[:, b, c, :], W[nm][l, c * 128:(c + 1) * 128, :], w=[b_wbr[b]])
                for c in range(8):
                    k.dma(pool, WO[:, c, :], W['w_out'][l, c * 128:(c + 1) * 128, :], w=[b_wo])
                npipe = NormPipe(st, src, W['mix_norm'][l, :], [5])
                yin = [sbt(st, f"yin{i}", [128, 1536], BF16) for i in range(2)]; b_yin = [Buf(), Buf()]
                yT = sbt(st, "yT", [128, 12, 128], BF16); b_yT = Buf()
                sg = [sbt(st, f"sg{i}", [128, 512], F32) for i in range(2)]; b_sg = [Buf(), Buf()]
                mg = sbt(st, "mg", [128, 512], F32); b_mg = Buf()
                mt = sbt(st, "mt", [128, 512], F32); b_mt = Buf()
                mb = sbt(st, "mb", [128, D], BF16); b_mb = Buf()
                mT = sbt(st, "mT", [128, 8, 128], BF16); b_mT = Buf()
                xr = [sbt(st, f"xr{i}", [128, D], F32) for i in range(2)]; b_xr = [Buf(), Buf()]
                src_ap, src_key = src; dst_ap, dst_key = dst
                mbs = [mb, sbt(st, "mb2", [128, D], BF16)]; b_mbs = [b_mb, Buf()]
                ng_ = [0]
                hT = npipe.hT
                t6 = ps[6][:].bitcast(BF16); t7 = ps[7][:].bitcast(BF16); t5 = ps[5][:].bitcast(BF16)

                def SA(tt):
                    t = tt % 4; g = tt // 4
                    ysl = tt % 2
                    k.dma(sp, yin[ysl][:, 0:512], ya_d[:, tt, :], r=[k.dbuf(('ya', g))], w=[b_yin[ysl]])
                    k.dma(sp, yin[ysl][:, 512:1536], ybc_d[:, tt, :], r=[k.dbuf(('ybc', tt))], w=[b_yin[ysl]])
                    k.dma(sp, xr[ysl][:], src_ap[tt * 128:(tt + 1) * 128, :], r=[k.dbuf((src_key, tt))], w=[b_xr[ysl]])
                    for c in range(12):
                        dstp = t6[:, c * 128:(c + 1) * 128] if c < 8 else t7[:, (c - 8) * 128:(c - 7) * 128]
                        bkk = 6 if c < 8 else 7
                        k.op(pe, lambda: T.transpose(dstp, yin[ysl][:, c * 128:(c + 1) * 128], identb[:]), r=[b_yin[ysl], b_id], w=[psb[bkk]],
                             inc=(c == 7 or c == 11))
                    k.op(act, lambda: A.copy(out=yT[:, 0:8, :].rearrange("p a b -> p (a b)"), in_=t6), r=[psb[6]], w=[b_yT])
                    k.op(dve, lambda: V.tensor_copy(out=yT[:, 8:12, :].rearrange("p a b -> p (a b)"), in_=t7[:, 0:512]), r=[psb[7]], w=[b_yT])
                    for half in range(2):
                        for b in range(3):
                            gb = ng_[0] % 2; bb = 2 + ng_[0] % 2; ng_[0] += 1
                            for kk in range(8):
                                mm(ps[gb][:, :], hT[:, kk, t * 128:(t + 1) * 128], WG[:, kk, b * 1024 + half * 512:b * 1024 + (half + 1) * 512],
                                   r=[npipe.b_hT, b_wg[b * 2 + half]], w=[psb[gb]], start=(kk == 0), stop=(kk == 7), inc=(kk == 7))
                            k.op(act, lambda: A.activation(out=sg[gb][:], in_=ps[gb][:, :], func=AF.Sigmoid), r=[psb[gb]], w=[b_sg[gb]])
                            for kk in range(4):
                                mm(ps[bb][:, :], yT[:, b * 4 + kk, :], WBR[:, b, kk, half * 512:(half + 1) * 512], r=[b_yT, b_wbr[b]], w=[psb[bb]],
                                   start=(kk == 0), stop=(kk == 3), inc=(kk == 3))
                            if b == 0:
                                k.op(dve, lambda: V.tensor_tensor(out=mg[:], in0=sg[gb][:], in1=ps[bb][:, :], op=ALU.mult), r=[b_sg[gb], psb[bb]],
                                     w=[b_mg])
                            else:
                                k.op(dve, lambda: V.tensor_tensor(out=mt[:], in0=sg[gb][:], in1=ps[bb][:, :], op=ALU.mult), r=[b_sg[gb], psb[bb]],
                                     w=[b_mt])
                                if b == 1:
                                    k.op(pool, lambda: G.tensor_tensor(out=mg[:], in0=mg[:], in1=mt[:], op=ALU.add), r=[b_mg, b_mt], w=[b_mg])
                                else:
                                    k.op(pool, lambda: G.tensor_tensor(out=mbs[ysl][:, half * 512:(half + 1) * 512], in0=mg[:], in1=mt[:], op=ALU.add),
                                         r=[b_mg, b_mt], w=[b_mbs[ysl]])

                def SB(tt):
                    ysl = tt % 2
                    for c in range(8):
                        k.op(pe, lambda: T.transpose(t5[:, c * 128:(c + 1) * 128], mbs[ysl][:, c * 128:(c + 1) * 128], identb[:]),
                             r=[b_mbs[ysl], b_id], w=[psb[5]], inc=(c == 7))
                    k.op(act, lambda: A.copy(out=mT[:].rearrange("p a b -> p (a b)"), in_=t5), r=[psb[5]], w=[b_mT])
                    for half in range(2):
                        for kk in range(8):
                            mm(ps[4][:, :], mT[:, kk, :], WO[:, kk, half * 512:(half + 1) * 512], r=[b_mT, b_wo], w=[psb[4]], start=(kk == 0),
                               stop=(kk == 7), inc=(kk == 7))
                        k.op(dve, lambda: V.tensor_tensor(out=xr[ysl][:, half * 512:(half + 1) * 512], in0=ps[4][:, :],
                                                          in1=xr[ysl][:, half * 512:(half + 1) * 512], op=ALU.add), r=[psb[4], b_xr[ysl]],
                             w=[b_xr[ysl]])
                    k.dma(sp, dst_ap[tt * 128:(tt + 1) * 128, :], xr[ysl][:], r=[b_xr[ysl]], w=[k.dbuf((dst_key, tt))])

                npipe.part1(0); npipe.part2(0)
                for tt in range(NT + 1):
                    g, t = tt // 4, tt % 4
                    if tt < NT:
                        if t == 0 and tt > 0:
                            npipe.part2(g)
                        SA(tt)
                        if t == 1 and g + 1 < NG:
                            npipe.part1(g + 1)
                    if tt >= 1:
                        SB(tt - 1)
                k.barrier()

        k.barrier()
        XR = (xres, 'xres')
        for l in range(DEPTH):
            if PH is None or 'ffn1' in PH: ffn_phase(l, 1, (x_in, 'x') if l == 0 else XR, XR, False)
            if PH is None or 'm1a' in PH: m1a_phase(l, XR)
            if PH is None or 'attn' in PH: attn_phase(l)
            if PH is None or 'm1b' in PH: m1b_phase(l, XR)
            if PH is None or 'm1c' in PH: m1c_phase(l, XR, XR)
            if PH is None or 'ffn2' in PH: ffn_phase(l, 2, XR, XR, l == DEPTH - 1)
        k.barrier()
    return nc


def make_consts(S):
    NT = S // 128
    c = {}
    c['c_ident'] = np.eye(128, dtype=np.float32)
    d = 64
    inv = (10000.0 ** (-np.arange(0, d, 2, dtype=np.float32) / np.float32(d))).astype(np.float32)
    ang = (np.arange(S, dtype=np.float32)[:, None] * inv[None, :]).astype(np.float32)
    cs = np.concatenate([np.cos(ang), np.sin(ang)], axis=1).astype(np.float32)
    c['c_rope'] = np.ascontiguousarray(cs.reshape(NT, 128, 64).transpose(1, 0, 2))
    h = np.arange(4, dtype=np.float64)
    log_g = np.log1p(-np.exp2(-5.0 - h))
    pos = np.arange(128, dtype=np.float64)
    eq = np.exp(log_g[None, :] * (pos[:, None] + 1.0))
    ek = np.exp(-log_g[None, :] * (pos[:, None] + 1.0)) * 0.125
    rt = np.concatenate([np.repeat(eq, 64, axis=1), np.repeat(ek, 64, axis=1)], axis=1)
    c['c_rtab'] = rt.astype(np.float32)
    j = np.arange(128)
    up = (j[:, None] <= j[None, :])
    c['c_tri'] = (up * (-1.0 / 16.0)).astype(np.float32)
    c['c_mask'] = up.astype(np.float32)
    rd = np.zeros((128, 2), np.float64)
    for pr in range(2):
        for hp in range(2):
            rd[hp * 64:(hp + 1) * 64, pr] = np.exp(log_g[2 * pr + hp] * 128.0)
    c['c_rdec'] = rd.astype(np.float32)
    return c


_CACHE = {}


def kernel(**inputs):
    S, DEPTH, B = 8192, 4, 4
    if 'nc' not in _CACHE:
        _CACHE['nc'] = build(S, DEPTH)
    nc = _CACHE['nc']
    consts = make_consts(S)
    x = np.ascontiguousarray(np.asarray(inputs['x'], dtype=np.float32))
    shared = {n: np.ascontiguousarray(np.asarray(inputs[n], dtype=np.float32)) for n, _ in WSPEC}
    shared['final_norm'] = np.ascontiguousarray(np.asarray(inputs['final_norm'], dtype=np.float32))
    shared.update(consts)
    in_maps = []
    for c in range(B):
        m = dict(shared); m['x'] = x[c]
        in_maps.append(m)
    res = run_bass_kernel_spmd(nc, in_maps, core_ids=list(range(B)))
    return np.stack([np.asarray(r['y']).reshape(S, D) for r in res.results], axis=0).astype(np.float32)
```
